# Optimizing a Trainium2 kernel written in Bass

```python
import jax, jax.numpy as jnp
from jax import lax
import numpy as np

D_MODEL = 2048
BATCH = 16
SEQ = 2048
DEPTH = 4

CTX_LEN = 256
GRID_W = 64
N_MIXERS = 4
EPS = 1e-6
NEG = -1e30
CHUNK = 64
ROPE_THETA = 10000.0
RET_HEADS = 8
RET_DK = D_MODEL // RET_HEADS
RET_DV = D_MODEL // RET_HEADS
ML_HEADS = 8
ML_DK = D_MODEL // (2 * ML_HEADS)
ML_DV = D_MODEL // ML_HEADS
NA_HEADS = 16
NA_DH = D_MODEL // NA_HEADS
NA_WIN_R = 8
NA_WIN_C = 16
NA_QB = 8
NA_KB = NA_QB + NA_WIN_C
HG_HEADS = 16
HG_DK = D_MODEL // HG_HEADS
HG_DV = D_MODEL // HG_HEADS
N_EXPERTS = 16
EXPERT_FF = 1024
EC_CAPACITY = 2

kernel_name = "hybrid_ret_mlstm_na_hgrn2_ecmoe_dit"


def rmsnorm(x, g):
    xf = x.astype(jnp.float32)
    y = xf * lax.rsqrt(jnp.mean(xf * xf, -1, keepdims=True) + EPS)
    return (y * g.astype(jnp.float32)).astype(x.dtype)


def modulate(h, shift, scale):
    return h * (1.0 + scale) + shift


def head_norm(o, w, b=None, center=False):
    B, T = o.shape[:2]
    o = o.astype(jnp.float32)
    if center:
        o = o - jnp.mean(o, -1, keepdims=True)
    o = o * lax.rsqrt(jnp.mean(o * o, -1, keepdims=True) + EPS)
    o = o.reshape(B, T, -1) * w.astype(jnp.float32)
    if b is not None:
        o = o + b.astype(jnp.float32)
    return o


def axial_rope(x):
    T, dh = x.shape[1], x.shape[-1]
    t = jnp.arange(T)
    row = (t // GRID_W).astype(jnp.float32)
    col = (t % GRID_W).astype(jnp.float32)
    quarter = dh // 4
    inv = ROPE_THETA ** (-jnp.arange(quarter, dtype=jnp.float32) / quarter)
    ang = jnp.concatenate([row[:, None] * inv, col[:, None] * inv], -1)
    cos = jnp.cos(ang)[None, :, None, :]
    sin = jnp.sin(ang)[None, :, None, :]
    xf = x.astype(jnp.float32)
    x1, x2 = xf[..., : dh // 2], xf[..., dh // 2:]
    return jnp.concatenate([x1 * cos - x2 * sin, x1 * sin + x2 * cos], -1).astype(x.dtype)


def _to_chunks(a, cs):
    B, T = a.shape[:2]
    a = a.astype(jnp.float32).reshape(B, T // cs, cs, *a.shape[2:])
    return jnp.swapaxes(jnp.moveaxis(a, 1, 0), 2, 3)


def chunk_gla(q, k, v, log_f, state):
    B, T, H, _ = q.shape
    cs = min(CHUNK, T)
    qc, kc, vc, gc = (_to_chunks(a, cs) for a in (q, k, v, log_f))
    b = jnp.cumsum(gc, axis=3)
    b_end = b[:, :, :, -1:, :]
    q_in = qc * jnp.exp(b)
    k_in = kc * jnp.exp(-b)
    k_end = kc * jnp.exp(b_end - b)
    decay_end = jnp.exp(b_end[:, :, :, 0, :])
    lower = jnp.tril(jnp.ones((cs, cs), bool))

    def step(S, xs):
        qi, ki, ke, vi, de = xs
        A = jnp.where(lower, jnp.einsum('bhik,bhjk->bhij', qi, ki), 0.0)
        o = jnp.einsum('bhij,bhjv->bhiv', A, vi) + jnp.einsum('bhik,bhkv->bhiv', qi, S)
        S = de[..., None] * S + jnp.einsum('bhjk,bhjv->bhkv', ke, vi)
        return S, o

    S, o = lax.scan(step, state, (q_in, k_in, k_end, vc, decay_end))
    return o.transpose(1, 0, 3, 2, 4).reshape(B, T, H, -1), S


def chunk_mlstm(q, k, v, log_i, log_f, state):
    B, T, H, _ = q.shape
    cs = min(CHUNK, T)
    qc, kc, vc, ic, fc = (_to_chunks(a, cs) for a in (q, k, v, log_i, log_f))
    bc = jnp.cumsum(fc, axis=-1)
    lower = jnp.tril(jnp.ones((cs, cs), bool))

    def step(carry, xs):
        C, n, m = carry
        qi, ki, vi, ii, bi = xs
        logw = jnp.where(lower, bi[..., :, None] - bi[..., None, :] + ii[..., None, :], -jnp.inf)
        log_carry = bi + m[..., None]
        m_i = jnp.maximum(log_carry, jnp.max(logw, -1))
        s = jnp.einsum('bhid,bhjd->bhij', qi, ki) * jnp.exp(logw - m_i[..., None])
        a = jnp.exp(log_carry - m_i)
        num = jnp.einsum('bhij,bhjv->bhiv', s, vi) + a[..., None] * jnp.einsum('bhid,bhdv->bhiv', qi, C)
        den = jnp.sum(s, -1) + a * jnp.einsum('bhid,bhd->bhi', qi, n)
        h = num / jnp.maximum(jnp.abs(den), jnp.exp(-m_i))[..., None]
        b_end = bi[..., -1]
        logw_end = b_end[..., None] - bi + ii
        m_new = jnp.maximum(b_end + m, jnp.max(logw_end, -1))
        decay = jnp.exp(b_end + m - m_new)
        w_end = jnp.exp(logw_end - m_new[..., None])
        C = decay[..., None, None] * C + jnp.einsum('bhj,bhjd,bhjv->bhdv', w_end, ki, vi)
        n = decay[..., None] * n + jnp.einsum('bhj,bhjd->bhd', w_end, ki)
        return (C, n, m_new), h

    state, h = lax.scan(step, state, (qc, kc, vc, ic, bc))
    return h.transpose(1, 0, 3, 2, 4).reshape(B, T, H, -1), state


def bidirectional(scan, ctx_fwd, lat_fwd, ctx_bwd, lat_bwd, state0):
    rev = lambda args: tuple(a[:, ::-1] for a in args)
    oc_f, sc_f = scan(*ctx_fwd, state0)
    ol_f, _ = scan(*lat_fwd, sc_f)
    oc_b, sc_b = scan(*rev(ctx_bwd), state0)
    ol_b, _ = scan(*rev(lat_bwd), sc_b)
    return oc_f + oc_b[:, ::-1], ol_f + ol_b[:, ::-1]


def retention_mixer(a_ctx, a_lat, wq, wk, wv, wg, wo, decay, gn_w, gn_b):
    log_gamma = jax.nn.log_sigmoid(decay.astype(jnp.float32))

    def proj(a, grid):
        B, T, _ = a.shape
        q = (a @ wq).reshape(B, T, RET_HEADS, RET_DK)
        k = (a @ wk).reshape(B, T, RET_HEADS, RET_DK) * RET_DK ** -0.5
        v = (a @ wv).reshape(B, T, RET_HEADS, RET_DV)
        if grid:
            q, k = axial_rope(q), axial_rope(k)
        lg = [jnp.broadcast_to(log_gamma[d][:, None], (B, T, RET_HEADS, RET_DK)) for d in range(2)]
        return (q, k, v, lg[0]), (q, k, v, lg[1])

    ctx_f, ctx_b = proj(a_ctx, False)
    lat_f, lat_b = proj(a_lat, True)
    s0 = jnp.zeros((a_lat.shape[0], RET_HEADS, RET_DK, RET_DV), jnp.float32)
    o_ctx, o_lat = bidirectional(chunk_gla, ctx_f, lat_f, ctx_b, lat_b, s0)

    def out(a, o):
        o = head_norm(o, gn_w, gn_b, center=True)
        return (jax.nn.silu(a @ wg) * o).astype(a.dtype) @ wo

    return out(a_ctx, o_ctx), out(a_lat, o_lat)


def mlstm_mixer(a_ctx, a_lat, wq, wk, wv, wog, wgate, bgate, norm_w, wout):
    def proj(a):
        B, T, _ = a.shape
        q = (a @ wq).reshape(B, T, ML_HEADS, ML_DK)
        k = (a @ wk).reshape(B, T, ML_HEADS, ML_DK) * ML_DK ** -0.5
        v = (a @ wv).reshape(B, T, ML_HEADS, ML_DV)
        dirs = []
        for d in range(2):
            g = (jnp.einsum('btd,dg->btg', a, wgate[d]) + bgate[d]).astype(jnp.float32)
            dirs.append((q, k, v, g[..., :ML_HEADS], jax.nn.log_sigmoid(g[..., ML_HEADS:])))
        return dirs

    ctx_f, ctx_b = proj(a_ctx)
    lat_f, lat_b = proj(a_lat)
    B = a_lat.shape[0]
    s0 = (jnp.zeros((B, ML_HEADS, ML_DK, ML_DV), jnp.float32),
          jnp.zeros((B, ML_HEADS, ML_DK), jnp.float32),
          jnp.zeros((B, ML_HEADS), jnp.float32))
    o_ctx, o_lat = bidirectional(chunk_mlstm, ctx_f, lat_f, ctx_b, lat_b, s0)

    def out(a, o):
        o = head_norm(o, norm_w, center=True)
        return (jax.nn.sigmoid(a @ wog) * o).astype(a.dtype) @ wout

    return out(a_ctx, o_ctx), out(a_lat, o_lat)


def _na_tables(rows):
    wr = min(NA_WIN_R, rows)
    nb = GRID_W // NA_QB
    qr = np.arange(rows)
    r0 = np.clip(qr - wr // 2, 0, rows - wr)
    blk = np.arange(nb)
    s0 = np.clip(blk * NA_QB - NA_WIN_C // 2, 0, GRID_W - NA_KB)
    krow = r0[:, None] + np.arange(wr)[None, :]
    kcol = s0[:, None] + np.arange(NA_KB)[None, :]
    krow_f = np.broadcast_to(krow[:, None, :, None], (rows, nb, wr, NA_KB)).reshape(rows, nb, -1)
    kcol_f = np.broadcast_to(kcol[None, :, None, :], (rows, nb, wr, NA_KB)).reshape(rows, nb, -1)
    key_idx = krow_f * GRID_W + kcol_f
    qcol = blk[:, None] * NA_QB + np.arange(NA_QB)[None, :]
    c0 = np.clip(qcol - NA_WIN_C // 2, 0, GRID_W - NA_WIN_C)
    kc4 = kcol_f[:, :, None, :]
    col_ok = (kc4 >= c0[None, :, :, None]) & (kc4 < c0[None, :, :, None] + NA_WIN_C)
    dr = krow_f[:, :, None, :] - qr[:, None, None, None]
    dc = np.clip(kc4 - qcol[None, :, :, None], -(NA_WIN_C - 1), NA_WIN_C - 1)
    rel_idx = (dr + NA_WIN_R - 1) * (2 * NA_WIN_C - 1) + (dc + NA_WIN_C - 1)
    return key_idx.astype(np.int32), col_ok, rel_idx.astype(np.int32)


def na_mixer(a_ctx, a_lat, wqkv, rpb, wo):
    B, N, D = a_lat.shape
    L = a_ctx.shape[1]
    rows = N // GRID_W
    nb = GRID_W // NA_QB
    scale = NA_DH ** -0.5

    def heads(a):
        T = a.shape[1]
        qkv = (a @ wqkv).reshape(B, T, 3, NA_HEADS, NA_DH).transpose(2, 0, 3, 1, 4)
        return qkv[0], qkv[1], qkv[2]

    qc, kc, vc = heads(a_ctx)
    ql, kl, vl = heads(a_lat)
    pc = jax.nn.softmax(jnp.einsum('bhqd,bhkd->bhqk', qc, kc).astype(jnp.float32) * scale, -1)
    o_ctx = jnp.einsum('bhqk,bhkd->bhqd', pc.astype(vc.dtype), vc).transpose(0, 2, 1, 3).reshape(B, L, D)
    key_idx, col_ok, rel_idx = _na_tables(rows)
    bias = rpb.reshape(NA_HEADS, -1).astype(jnp.float32)[:, rel_idx]
    bias = jnp.where(jnp.asarray(col_ok)[None], bias, NEG).transpose(1, 0, 2, 3, 4)
    q_rows = ql.reshape(B, NA_HEADS, rows, nb, NA_QB, NA_DH).transpose(2, 0, 1, 3, 4, 5)
    n_win = key_idx.shape[-1]

    def row_attn(xs):
        qr, kidx, br = xs
        kg = kl[:, :, kidx]
        vg = vl[:, :, kidx]
        s_win = jnp.einsum('bhjqd,bhjkd->bhjqk', qr, kg).astype(jnp.float32) * scale + br[None]
        s_ctx = jnp.einsum('bhjqd,bhld->bhjql', qr, kc).astype(jnp.float32) * scale
        p = jax.nn.softmax(jnp.concatenate([s_win, s_ctx], -1), -1).astype(vl.dtype)
        return (jnp.einsum('bhjqk,bhjkd->bhjqd', p[..., :n_win], vg)
                + jnp.einsum('bhjql,bhld->bhjqd', p[..., n_win:], vc))

    o_rows = lax.map(row_attn, (q_rows, jnp.asarray(key_idx), bias))
    o_lat = o_rows.transpose(1, 0, 3, 4, 2, 5).reshape(B, N, D)
    return o_ctx @ wo, o_lat @ wo


def hgrn2_mixer(a_ctx, a_lat, wq, wi, wf, wg, norm_w, wo, lb):
    lb = lb.reshape(HG_HEADS, HG_DK)

    def proj(a):
        B, T, _ = a.shape
        q = jax.nn.silu(a @ wq).reshape(B, T, HG_HEADS, HG_DK) * HG_DK ** -0.5
        v = (a @ wi).reshape(B, T, HG_HEADS, HG_DV)
        dirs = []
        for d in range(2):
            z = (a @ wf[d]).reshape(B, T, HG_HEADS, HG_DK).astype(jnp.float32)
            f = lb + (1.0 - lb) * jax.nn.sigmoid(z)
            dirs.append((q, (1.0 - lb) * jax.nn.sigmoid(-z), v, jnp.log(f)))
        return dirs

    ctx_f, ctx_b = proj(a_ctx)
    lat_f, lat_b = proj(a_lat)
    s0 = jnp.zeros((a_lat.shape[0], HG_HEADS, HG_DK, HG_DV), jnp.float32)
    o_ctx, o_lat = bidirectional(chunk_gla, ctx_f, lat_f, ctx_b, lat_b, s0)

    def out(a, o):
        o = head_norm(o, norm_w)
        return (jax.nn.silu(a @ wg) * o).astype(a.dtype) @ wo

    return out(a_ctx, o_ctx), out(a_lat, o_lat)


def expert_choice_ffn(h, w_router, w1, w3, w2):
    B, n, D = h.shape
    cap = max(1, EC_CAPACITY * n // N_EXPERTS)
    aff = jax.nn.softmax(jnp.einsum('bnd,de->bne', h, w_router).astype(jnp.float32), axis=-1)
    gate, idx = lax.top_k(jnp.swapaxes(aff, 1, 2), cap)
    xg = jax.vmap(lambda hb, ib: hb[ib])(h, idx)
    u = jnp.einsum('becd,edf->becf', xg, w1)
    g = jnp.einsum('becd,edf->becf', xg, w3)
    y = jnp.einsum('becf,efd->becd', jax.nn.silu(u) * g, w2) * gate[..., None].astype(h.dtype)
    return jax.vmap(lambda yb, ib: jnp.zeros((n, D), yb.dtype).at[ib.reshape(-1)].add(yb.reshape(-1, D)))(y, idx)


def setup_inputs(seed: int = 0) -> dict:
    key = jax.random.key(seed)
    ks = iter(jax.random.split(key, 48))
    D = D_MODEL

    def nrm(shape, scale=1.0):
        return scale * jax.random.normal(next(ks), shape, jnp.float32)

    def dense(shape, fan_in, gain=1.0):
        return nrm(shape, gain * fan_in ** -0.5)

    gam = 1.0 - 2.0 ** (-5.0 - jnp.arange(RET_HEADS, dtype=jnp.float32))
    return {
        'x': nrm((BATCH, SEQ, D)),
        'c': nrm((BATCH, D)),
        'ctx': nrm((BATCH, CTX_LEN, D)),
        'c_ctx': nrm((D,)),
        'ada_w': dense((DEPTH, D, 6 * D), D, 0.5),
        'ada_b': nrm((DEPTH, 6 * D), 0.02),
        'norm_g': 1.0 + nrm((DEPTH, 2, D), 0.02),
        'final_g': 1.0 + nrm((D,), 0.02),
        'ret_wq': dense((D, RET_HEADS * RET_DK), D),
        'ret_wk': dense((D, RET_HEADS * RET_DK), D),
        'ret_wv': dense((D, RET_HEADS * RET_DV), D),
        'ret_wg': dense((D, RET_HEADS * RET_DV), D),
        'ret_wo': dense((RET_HEADS * RET_DV, D), RET_HEADS * RET_DV),
        'ret_decay': (jnp.log(gam) - jnp.log1p(-gam))[None, :] + nrm((2, RET_HEADS), 0.01),
        'ret_gn_w': 1.0 + nrm((D,), 0.02),
        'ret_gn_b': nrm((D,), 0.02),
        'ml_wq': dense((D, ML_HEADS * ML_DK), D),
        'ml_wk': dense((D, ML_HEADS * ML_DK), D),
        'ml_wv': dense((D, ML_HEADS * ML_DV), D),
        'ml_wog': dense((D, ML_HEADS * ML_DV), D),
        'ml_wgate': dense((2, D, 2 * ML_HEADS), D, 0.5),
        'ml_bgate': jnp.concatenate([nrm((2, ML_HEADS), 0.1),
                                     jnp.linspace(3.0, 6.0, ML_HEADS, dtype=jnp.float32)[None, :]
                                     + nrm((2, ML_HEADS), 0.01)], axis=-1),
        'ml_norm_w': 1.0 + nrm((D,), 0.02),
        'ml_wout': dense((ML_HEADS * ML_DV, D), ML_HEADS * ML_DV),
        'na_wqkv': dense((D, 3 * D), D),
        'na_rpb': nrm((NA_HEADS, 2 * NA_WIN_R - 1, 2 * NA_WIN_C - 1), 0.02),
        'na_wo': dense((D, D), D),
        'hg_wq': dense((D, HG_HEADS * HG_DK), D),
        'hg_wi': dense((D, HG_HEADS * HG_DV), D),
        'hg_wf': dense((2, D, HG_HEADS * HG_DK), D),
        'hg_wg': dense((D, HG_HEADS * HG_DV), D),
        'hg_norm_w': 1.0 + nrm((D,), 0.02),
        'hg_wo': dense((HG_HEADS * HG_DV, D), HG_HEADS * HG_DV),
        'hg_lb': nrm((DEPTH, HG_HEADS * HG_DK), 0.1),
        'moe_router': dense((DEPTH, D, N_EXPERTS), D),
        'moe_w1': dense((DEPTH, N_EXPERTS, D, EXPERT_FF), D),
        'moe_w3': dense((DEPTH, N_EXPERTS, D, EXPERT_FF), D),
        'moe_w2': dense((DEPTH, N_EXPERTS, EXPERT_FF, D), EXPERT_FF),
    }


def reference(x, c, ctx, c_ctx, ada_w, ada_b, norm_g, final_g,
              ret_wq, ret_wk, ret_wv, ret_wg, ret_wo, ret_decay, ret_gn_w, ret_gn_b,
              ml_wq, ml_wk, ml_wv, ml_wog, ml_wgate, ml_bgate, ml_norm_w, ml_wout,
              na_wqkv, na_rpb, na_wo,
              hg_wq, hg_wi, hg_wf, hg_wg, hg_norm_w, hg_wo, hg_lb,
              moe_router, moe_w1, moe_w3, moe_w2):
    h_lat, h_ctx = x, ctx
    s_lat = jax.nn.silu(c)
    s_ctx = jax.nn.silu(c_ctx)
    lb_all = jnp.cumsum(jax.nn.softmax(hg_lb.astype(jnp.float32), axis=0), axis=0)
    for i in range(DEPTH):
        last = i == DEPTH - 1
        sh1, sc1, g1, sh2, sc2, g2 = jnp.split((s_lat @ ada_w[i] + ada_b[i])[:, None, :], 6, axis=-1)
        csh1, csc1, cg1, csh2, csc2, cg2 = jnp.split(s_ctx @ ada_w[i] + ada_b[i], 6, axis=-1)
        a_lat = modulate(rmsnorm(h_lat, norm_g[i, 0]), sh1, sc1)
        a_ctx = modulate(rmsnorm(h_ctx, norm_g[i, 0]), csh1, csc1)
        kind = i % N_MIXERS
        if kind == 0:
            o_ctx, o_lat = retention_mixer(a_ctx, a_lat, ret_wq, ret_wk, ret_wv, ret_wg, ret_wo,
                                           ret_decay, ret_gn_w, ret_gn_b)
        elif kind == 1:
            o_ctx, o_lat = mlstm_mixer(a_ctx, a_lat, ml_wq, ml_wk, ml_wv, ml_wog, ml_wgate, ml_bgate,
                                       ml_norm_w, ml_wout)
        elif kind == 2:
            o_ctx, o_lat = na_mixer(a_ctx, a_lat, na_wqkv, na_rpb, na_wo)
        else:
            o_ctx, o_lat = hgrn2_mixer(a_ctx, a_lat, hg_wq, hg_wi, hg_wf, hg_wg, hg_norm_w, hg_wo,
                                       lb_all[i] - lb_all[0])
        h_lat = h_lat + g1 * o_lat.astype(h_lat.dtype)
        f_lat = modulate(rmsnorm(h_lat, norm_g[i, 1]), sh2, sc2)
        h_lat = h_lat + g2 * expert_choice_ffn(f_lat, moe_router[i], moe_w1[i], moe_w3[i], moe_w2[i])
        if not last:
            h_ctx = h_ctx + cg1 * o_ctx.astype(h_ctx.dtype)
            f_ctx = modulate(rmsnorm(h_ctx, norm_g[i, 1]), csh2, csc2)
            h_ctx = h_ctx + cg2 * expert_choice_ffn(f_ctx, moe_router[i], moe_w1[i], moe_w3[i], moe_w2[i])
    return rmsnorm(h_lat, final_g)
```

```python
import numpy as np
from contextlib import ExitStack
import concourse.bass as bass
import concourse.mybir as mybir
from concourse.bass_utils import run_bass_kernel_spmd

F32 = mybir.dt.float32
F32R = mybir.dt.float32r
AF = mybir.ActivationFunctionType
ALU = mybir.AluOpType
AX = mybir.AxisListType

D = 2048
NCH = 16
LCTX = 256
NLAT = 2048
T = LCTX + NLAT
NB = 2
NCORES = 8
TILES = [(0, 256), (256, 512), (768, 512), (1280, 512), (1792, 512)]
EPS = 1e-6
SAME_SYNC = True
RING = 12
FAST = False


class Buf:
    __slots__ = ("name", "multi", "writers", "readers", "psum")

    def __init__(self, name, multi=False):
        self.name = name
        self.multi = multi
        self.psum = name.startswith("bank")
        self.writers = []
        self.readers = []


class V:
    __slots__ = ("ap", "buf")

    def __init__(self, ap, buf):
        self.ap = ap
        self.buf = buf

    def __getitem__(self, k):
        return V(self.ap[k], self.buf)

    def rr(self, pat, **kw):
        return V(self.ap.rearrange(pat, **kw), self.buf)

    def bc(self, shape):
        return V(self.ap.to_broadcast(list(shape)), self.buf)

    def un(self, axis):
        return V(self.ap.unsqueeze(axis), self.buf)

    def wb(self, buf):
        return V(self.ap, buf)

    @property
    def shape(self):
        return self.ap.shape


class Op:
    __slots__ = ("eng", "fn", "deps", "is_dma", "ms", "sem", "val", "prev")


def _mmap(v):
    if FAST:
        return v.ap.bitcast(F32R)
    return v.ap


class Prog:
    ENGS = ("sp", "act", "dve", "pool", "pe")

    def __init__(self, nc, es):
        self.nc = nc
        self.ops = {e: [] for e in self.ENGS}
        self.bufs = []
        self.dma_since = []
        self.final = []
        self.sb = es.enter_context(nc.sbuf_tensor("sb", [128, 49152], F32))
        self.ps = es.enter_context(nc.psum_tensor("ps", [128, 4096], F32))
        self.sb_top = 0
        self.sb_base = 0
        self.sb_ptr = 0
        self.banks = [V(self.ps[:, i * 512:(i + 1) * 512], self.newbuf(f"bank{i}")) for i in range(8)]
        self.bank_rr = 0
        self.nops = 0

    def newbuf(self, name, multi=False):
        b = Buf(name, multi)
        self.bufs.append(b)
        return b

    def _alloc(self, shape, persistent):
        n = int(np.prod(shape))
        if persistent:
            assert self.sb_ptr == self.sb_base, "persistent alloc only between phases"
            off = self.sb_base
            self.sb_base += n
            self.sb_ptr = self.sb_base
        else:
            off = self.sb_ptr
            self.sb_ptr += n
        assert self.sb_ptr <= 49152, f"SBUF overflow {self.sb_ptr}"
        ap = self.sb[:, off:off + n]
        if len(shape) == 2:
            ap = ap.rearrange("p (a b) -> p a b", b=shape[1])
        elif len(shape) == 3:
            ap = ap.rearrange("p (a b c) -> p a b c", b=shape[1], c=shape[2])
        return ap

    def tile(self, name, shape, persistent=False, nparts=128):
        ap = self._alloc(shape, persistent)
        if nparts != 128:
            ap = ap[0:nparts]
        return V(ap, self.newbuf(name))

    def bank(self):
        b = self.banks[self.bank_rr % 8]
        self.bank_rr += 1
        return b

    def dram(self, name, shape, kind="Internal", tracked=True, multi=True):
        ap = self.nc.dram_tensor(name, list(shape), F32, kind=kind).ap()
        return V(ap, self.newbuf(name, multi=multi) if tracked else None)

    def _add(self, eng, fn, reads, writes, is_dma=False):
        op = Op()
        op.eng = eng
        op.fn = fn
        op.is_dma = is_dma
        op.ms = False
        op.sem = None
        op.val = 0
        op.prev = 0
        deps = []
        wb = []
        for v in writes:
            b = v.buf
            if b is None or b in wb:
                continue
            wb.append(b)
        rb = []
        for v in reads:
            b = v.buf
            if b is None or b in wb or b in rb:
                continue
            rb.append(b)
        for b in rb:
            deps.extend(b.writers)
            if b.psum:
                deps.extend(r_ for r_ in b.readers if r_.eng != eng)
        for b in wb:
            if b.multi:
                if b.readers:
                    deps.extend(b.readers)
                    deps.extend(b.writers)
                    b.writers = [op]
                    b.readers = []
                else:
                    self._push(b.writers, op)
            else:
                deps.extend(b.writers)
                deps.extend(b.readers)
                b.writers = [op]
                b.readers = []
        for b in rb:
            self._push(b.readers, op)
        dd = []
        for d in deps:
            if d is op or d in dd:
                continue
            if (not d.is_dma) and (not is_dma) and d.eng == eng and (eng == "pe" or not SAME_SYNC):
                continue
            d.ms = True
            dd.append(d)
        op.deps = dd
        self.ops[eng].append(op)
        self.nops += 1
        if is_dma:
            self.dma_since.append(op)
        return op

    @staticmethod
    def _push(lst, op):
        if not op.is_dma:
            for i, o in enumerate(lst):
                if (not o.is_dma) and o.eng == op.eng:
                    lst[i] = op
                    return
        lst.append(op)

    def barrier(self, reset_alloc=True):
        lasts = []
        for e in self.ENGS:
            for o in reversed(self.ops[e]):
                if not o.is_dma and o.fn is not None:
                    lasts.append(o)
                    break
        deps = lasts + self.dma_since
        for d in deps:
            d.ms = True
        for e in self.ENGS:
            op = Op()
            op.eng = e
            op.fn = None
            op.is_dma = False
            op.ms = False
            op.sem = None
            op.val = 0
            op.prev = 0
            op.deps = [d for d in deps if d.is_dma or d.eng != e or (SAME_SYNC and e != "pe")]
            self.ops[e].append(op)
        self.dma_since = []
        for b in self.bufs:
            b.writers = []
            b.readers = []
        if reset_alloc:
            self.sb_ptr = self.sb_base

    def dma(self, q, out, in_, final=False):
        o, i = out.ap, in_.ap
        op = self._add(q, lambda e: e.dma_start(out=o, in_=i), [in_], [out], is_dma=True)
        if final:
            self.final.append(op)
        return op

    def mm(self, out, lhsT, rhs, start, stop, extra_reads=()):
        o, l, r = out.ap, _mmap(lhsT), _mmap(rhs)
        return self._add("pe", lambda e: e.matmul(o, l, r, start=start, stop=stop),
                         [lhsT, rhs] + list(extra_reads), [out])

    def mm32(self, out, lhsT, rhs, start, stop):
        o, l, r = out.ap, lhsT.ap, rhs.ap
        return self._add("pe", lambda e: e.matmul(o, l, r, start=start, stop=stop), [lhsT, rhs], [out])

    def transpose(self, out, in_, ident):
        o, i, d = out.ap, in_.ap, ident.ap
        return self._add("pe", lambda e: e.transpose(o, i, d), [in_, ident], [out])

    def act(self, out, in_, func, bias=None, scale=1.0, accum=None):
        o, i = out.ap, in_.ap
        reads = [in_]
        kw = {}
        if isinstance(bias, V):
            reads.append(bias)
            kw["bias"] = bias.ap
        elif bias is not None:
            kw["bias"] = float(bias)
        if isinstance(scale, V):
            reads.append(scale)
            kw["scale"] = scale.ap
        else:
            kw["scale"] = float(scale)
        writes = [out]
        if accum is not None:
            kw["accum_out"] = accum.ap
            writes.append(accum)
        return self._add("act", lambda e: e.activation(o, i, func, **kw), reads, writes)

    def tt(self, eng, out, a, b, op):
        o, x, y = out.ap, a.ap, b.ap
        return self._add(eng, lambda e: e.tensor_tensor(o, x, y, op), [a, b], [out])

    def ts(self, eng, out, a, s1, s2, op0, op1=None):
        o, x = out.ap, a.ap
        reads = [a]
        c1 = s1.ap if isinstance(s1, V) else float(s1)
        if isinstance(s1, V):
            reads.append(s1)
        c2 = None
        if s2 is not None:
            c2 = s2.ap if isinstance(s2, V) else float(s2)
            if isinstance(s2, V):
                reads.append(s2)
        if op1 is None:
            return self._add(eng, lambda e: e.tensor_scalar(o, x, c1, None, op0), reads, [out])
        return self._add(eng, lambda e: e.tensor_scalar(o, x, c1, c2, op0, op1), reads, [out])

    def stt(self, eng, out, a, s, b, op0, op1):
        o, x, y = out.ap, a.ap, b.ap
        reads = [a, b]
        c = s.ap if isinstance(s, V) else float(s)
        if isinstance(s, V):
            reads.append(s)
        return self._add(eng, lambda e: e.scalar_tensor_tensor(o, x, c, y, op0, op1), reads, [out])

    def copy(self, eng, out, in_):
        o, i = out.ap, in_.ap
        if eng == "act":
            return self._add("act", lambda e: e.activation(o, i, AF.Copy), [in_], [out])
        return self._add(eng, lambda e: e.tensor_copy(o, i), [in_], [out])

    def memset(self, eng, out, val):
        o = out.ap
        return self._add(eng, lambda e: e.memset(o, val), [], [out])

    def generic(self, eng, fn, reads, writes):
        return self._add(eng, fn, reads, writes)

    def finalize(self, es):
        nc = self.nc
        comp = ("act", "dve", "pool", "pe")
        sems = {e: es.enter_context(nc.semaphore(f"s_{e}")) for e in comp}
        rings = {q: [es.enter_context(nc.semaphore(f"r_{q}{i}")) for i in range(RING)]
                 for q in ("sp", "act", "pool")}
        for e in self.ENGS:
            cnt = 0
            n = 0
            for op in self.ops[e]:
                if op.fn is None:
                    continue
                if op.is_dma:
                    op.sem = rings[e][n % RING]
                    op.val = 16 * (n // RING + 1)
                    op.prev = 16 * (n // RING)
                    n += 1
                elif op.ms:
                    cnt += 1
                    op.sem = sems[e]
                    op.val = cnt
        block = es.enter_context(nc.Block())
        names = {"sp": "sync", "act": "scalar", "dve": "vector", "pool": "gpsimd", "pe": "tensor"}
        final = self.final

        def make(e):
            def body(engine):
                known = {}
                semobj = {}

                def need(d):
                    k = id(d.sem)
                    semobj[k] = d.sem
                    return k

                for op in self.ops[e]:
                    waits = {}
                    for d in op.deps:
                        k = need(d)
                        if waits.get(k, 0) < d.val:
                            waits[k] = d.val
                    if op.is_dma and op.prev > 0:
                        k = id(op.sem)
                        semobj[k] = op.sem
                        if waits.get(k, 0) < op.prev:
                            waits[k] = op.prev
                    for k, val in waits.items():
                        if known.get(k, 0) < val:
                            engine.wait_ge(semobj[k], val)
                            known[k] = val
                    if op.fn is None:
                        continue
                    ins = op.fn(engine)
                    if op.is_dma:
                        ins.then_inc(op.sem, 16)
                    elif op.ms:
                        ins.then_inc(op.sem, 1)
                if e == "sp":
                    for d in final:
                        k = need(d)
                        if known.get(k, 0) < d.val:
                            engine.wait_ge(d.sem, d.val)
                            known[k] = d.val
            return body

        for e in self.ENGS:
            getattr(block, names[e])(make(e))


class Model:
    def __init__(self, P, layers, dbg=None, nb=NB, first_in=None):
        self.P = P
        self.layers = layers
        self.dbg = dbg or {}
        self.nb = nb
        self.inputs = {}
        self.outputs = {}

    def ext_in(self, name, shape):
        self.inputs[name] = tuple(shape)
        return self.P.dram(name, shape, kind="ExternalInput", tracked=False)

    def ext_out(self, name, shape):
        self.outputs[name] = tuple(shape)
        return self.P.dram(name, shape, kind="ExternalOutput", tracked=True)

    def wtile(self):
        t = self.wt[self.wt_i % len(self.wt)]
        self.wt_i += 1
        return t

    def setup(self):
        P = self.P
        nb = self.nb
        self.hT0 = self.ext_in("hT0", [nb, D, T])
        if 3 in self.layers:
            self.hT = P.dram("hT", [nb, D, T])
        else:
            self.hT = self.ext_out("hT_out", [nb, D, T])
        self.oT = P.dram("oT", [nb, D, T])
        self.fT = P.dram("fT", [nb, D, T])
        self.Gd = P.dram("Gd", [nb, 16, T])
        self.cvec = self.ext_in("cvec", [128, 16, 4])
        self.ada_w = self.ext_in("ada_w", [len(self.layers), D, 6 * D])
        self.ada_bT = self.ext_in("ada_bT", [len(self.layers), 128, 96])
        self.normgT = self.ext_in("normgT", [len(self.layers), 2, 128, 16])
        self.final_gT = self.ext_in("final_gT", [128, 16])
        self.router = self.ext_in("router", [len(self.layers), D, 16])
        self.w1 = self.ext_in("moe_w1", [len(self.layers), 16, D, 1024])
        self.w3 = self.ext_in("moe_w3", [len(self.layers), 16, D, 1024])
        self.w2 = self.ext_in("moe_w2", [len(self.layers), 16, 1024, D])
        self.identd = self.ext_in("ident", [128, 128])
        if 3 in self.layers:
            self.outT = self.ext_out("outT", [nb, D, NLAT])
        kinds = set(li % 4 for li in self.layers)
        if 0 in kinds or 1 in kinds:
            self.distf = self.ext_in("dist_f", [T, T])
            self.distb = self.ext_in("dist_b", [T, T])
        self.ones = P.tile("ones", [128], persistent=True)
        self.ident = P.tile("ident", [128], persistent=True)
        self.s_sb = P.tile("s_sb", [16, 4], persistent=True)
        self.mod = P.tile("mod", [96, 4], persistent=True)
        self.gsc = [P.tile(f"gsc{i}", [16, 4], persistent=True) for i in range(2)]
        self.ng = P.tile("ng", [2, 16], persistent=True)
        self.fg = P.tile("fg", [16], persistent=True)
        self.affT = [P.tile(f"affT{b}", [T], persistent=True, nparts=16) for b in range(nb)]
        self.eps_t = P.tile("eps", [1], persistent=True)
        P.memset("dve", self.ones, 1.0)
        P.memset("dve", self.eps_t, EPS)
        P.dma("sp", self.ident, self.identd)
        P.dma("sp", self.fg, self.final_gT)
        ctmp = P.tile("ctmp", [16, 4])
        P.dma("sp", ctmp, self.cvec)
        P.act(self.s_sb, ctmp, AF.Silu)
        P.barrier()

    def phase_mods(self, li):
        P = self.P
        self.wt = [P.tile(f"wt{i}", [16, 512]) for i in range(2)]
        self.wt_i = 0
        bT = P.tile("bT", [96])
        P.dma("sp", bT, self.ada_bT[li])
        P.dma("sp", self.ng, self.normgT[li].rr("w p c -> p w c"))
        for blk in range(24):
            w = self.wtile()
            P.dma("sp", w, self.ada_w[li][:, blk * 512:(blk + 1) * 512].rr("(kc p) n -> p kc n", p=128))
            for j in range(4):
                oc = blk * 4 + j
                ps = P.bank()
                for kc in range(16):
                    P.mm32(ps[:, 0:4], w[:, kc, j * 128:(j + 1) * 128], self.s_sb[:, kc, :], kc == 0, kc == 15)
                P.ts("dve", self.mod[:, oc, :], ps[:, 0:4], bT[:, oc:oc + 1], None, ALU.add)
        for w_i, sc_idx in ((0, 1), (1, 4)):
            tmp = P.tile(f"gtmp{w_i}", [16, 4])
            P.ts("dve", tmp, self.mod[:, sc_idx * 16:(sc_idx + 1) * 16, :], 1.0, None, ALU.add)
            P.tt("dve", self.gsc[w_i], tmp, self.ng[:, w_i, :].un(2).bc([128, 16, 4]), ALU.mult)
        P.barrier()

    def rsqrt(self, out, in_, scale, tmp):
        P = self.P
        P.act(tmp, in_, AF.Sqrt, bias=self.eps_t, scale=scale)
        o, i = out.ap, tmp.ap
        P.generic("dve", lambda e: e.reciprocal(o, i), [tmp], [out])

    def modv(self, which, kc, j):
        return self.mod[:, which * 16 + kc, j:j + 1]

    def norm_mod(self, h, a, n, w_i, j, rs, plain_g=None):
        P = self.P
        P.act(a[:, :, 0:n], h[:, :, 0:n], AF.Square)
        ps = P.bank()
        for kc in range(16):
            P.mm32(ps[:, 0:n], self.ones, a[:, kc, 0:n], kc == 0, kc == 15)
        rstd = rs[:, 0, 0:n]
        self.rsqrt(rstd, ps[:, 0:n], 1.0 / D, rs[:, 1, 0:n])
        if plain_g is not None:
            for kc in range(16):
                P.stt("dve", a[:, kc, 0:n], h[:, kc, 0:n], plain_g[:, kc:kc + 1], rstd, ALU.mult, ALU.mult)
            return
        sh_idx = 0 if w_i == 0 else 3
        for kc in range(16):
            P.stt("dve", a[:, kc, 0:n], h[:, kc, 0:n], self.gsc[w_i][:, kc, j:j + 1], rstd, ALU.mult, ALU.mult)
            P.act(a[:, kc, 0:n], a[:, kc, 0:n], AF.Identity, bias=self.modv(sh_idx, kc, j), scale=1.0)

    def linear_fm(self, a, n, W, ocs, cb, kcs=16):
        P = self.P
        nblk = (ocs + 3) // 4
        for blk in range(nblk):
            w = self.wtile()
            ncol = min(512, ocs * 128 - blk * 512)
            P.dma("sp", w[:, 0:kcs, 0:ncol], W[:, blk * 512:blk * 512 + ncol].rr("(kc p) n -> p kc n", p=128))
            for j in range(ncol // 128):
                oc = blk * 4 + j
                ps = P.bank()
                for kc in range(kcs):
                    P.mm(ps[:, 0:n], w[:, kc, j * 128:(j + 1) * 128], a[:, kc, 0:n], kc == 0, kc == kcs - 1)
                cb(oc, ps)

    def linear_tm(self, a, n, W, ncols, cb, kcs=16):
        P = self.P
        for blk in range((ncols + 511) // 512):
            w = self.wtile()
            ncol = min(512, ncols - blk * 512)
            P.dma("sp", w[:, 0:kcs, 0:ncol], W[:, blk * 512:blk * 512 + ncol].rr("(kc p) n -> p kc n", p=128))
            for ts in range(n // 128):
                ps = P.bank()
                for kc in range(kcs):
                    P.mm(ps[:, 0:ncol], a[:, kc, ts * 128:(ts + 1) * 128], w[:, kc, 0:ncol], kc == 0, kc == kcs - 1)
                cb(blk, ts, ps)

    def setup_ret(self):
        self.ret = dict(
            wq=self.ext_in("ret_wq", [D, D]), wk=self.ext_in("ret_wk", [D, D]), wv=self.ext_in("ret_wv", [D, D]),
            wg=self.ext_in("ret_wg", [D, D]), wo=self.ext_in("ret_wo", [D, D]),
            decay_b=self.ext_in("ret_decay_b", [128, 16]), gnw=self.ext_in("ret_gn_wT", [128, 16]),
            gnb=self.ext_in("ret_gn_bT", [128, 16]), cos=self.ext_in("rope_cosT", [128, NLAT]),
            sin=self.ext_in("rope_sinT", [128, NLAT]))

    def alloc_qkv(self, dq):
        P = self.P
        nb = self.nb
        if not hasattr(self, "qT"):
            self.qT = P.dram("qT", [nb, D, T])
            self.kT = P.dram("kT", [nb, D, T])
            self.vv = P.dram("vv", [nb, T, D])
            self.gT = P.dram("gT", [nb, D, T])

    def proj_common(self, li, per_tile):
        P = self.P
        self.wt = [P.tile(f"wt{i}", [16, 512]) for i in range(2)]
        self.wt_i = 0
        h = P.tile("h", [16, 512])
        a = P.tile("a", [16, 512])
        rs = P.tile("rs", [2, 512])
        src = self.hT0 if li == self.layers[0] else self.hT
        for b in range(self.nb):
            for ti, (t0, n) in enumerate(TILES):
                P.dma("sp", h[:, :, 0:n], src[b][:, t0:t0 + n].rr("(kc p) t -> p kc t", p=128))
                j = 2 if ti == 0 else b
                self.norm_mod(h, a, n, 0, j, rs)
                per_tile(b, ti, t0, n, a)

    def store_fm(self, dst, b, oc0, noc, t0, n, stage):
        self.P.dma("pool", dst[b][oc0 * 128:(oc0 + noc) * 128, t0:t0 + n].rr("(j p) t -> p j t", p=128),
                   stage[:, 0:noc, 0:n])

    def phase_proj_ret(self, li):
        P = self.P
        R = self.ret
        self.alloc_qkv(D)
        stg = [P.tile(f"stg{i}", [4, 512]) for i in range(2)]
        stv = [P.tile(f"stv{i}", [512]) for i in range(2)]
        cs = P.tile("cs", [2, 512])
        xs = P.tile("xs", [2, 512])
        tm = P.tile("tm", [4, 512])
        st_i = [0, 0]

        def per_tile(b, ti, t0, n, a):
            lat = ti > 0
            if lat:
                P.dma("sp", cs[:, 0, 0:n], R["cos"][:, t0 - LCTX:t0 - LCTX + n])
                P.dma("sp", cs[:, 1, 0:n], R["sin"][:, t0 - LCTX:t0 - LCTX + n])
            for W, dst, scale in ((R["wq"], self.qT, 1.0), (R["wk"], self.kT, 1.0 / 16.0)):
                def cb(oc, ps, dst=dst, scale=scale):
                    j = oc % 4
                    if j == 0:
                        st_i[0] += 1
                    stage = stg[st_i[0] % 2]
                    if not lat:
                        P.act(stage[:, j, 0:n], ps[:, 0:n], AF.Copy, scale=scale)
                    else:
                        half = oc % 2
                        P.act(xs[:, half, 0:n], ps[:, 0:n], AF.Copy, scale=scale)
                        if half == 1:
                            c_, s_ = cs[:, 0, 0:n], cs[:, 1, 0:n]
                            x1, x2 = xs[:, 0, 0:n], xs[:, 1, 0:n]
                            P.tt("dve", tm[:, 0, 0:n], x1, c_, ALU.mult)
                            P.tt("pool", tm[:, 1, 0:n], x2, s_, ALU.mult)
                            P.tt("dve", stage[:, j - 1, 0:n], tm[:, 0, 0:n], tm[:, 1, 0:n], ALU.subtract)
                            P.tt("dve", tm[:, 2, 0:n], x1, s_, ALU.mult)
                            P.tt("pool", tm[:, 3, 0:n], x2, c_, ALU.mult)
                            P.tt("dve", stage[:, j, 0:n], tm[:, 2, 0:n], tm[:, 3, 0:n], ALU.add)
                    if j == 3:
                        self.store_fm(dst, b, oc - 3, 4, t0, n, stage)
                self.linear_fm(a, n, W, 16, cb)

            def cbg(oc, ps):
                j = oc % 4
                if j == 0:
                    st_i[0] += 1
                stage = stg[st_i[0] % 2]
                P.act(stage[:, j, 0:n], ps[:, 0:n], AF.Silu)
                if j == 3:
                    self.store_fm(self.gT, b, oc - 3, 4, t0, n, stage)
            self.linear_fm(a, n, R["wg"], 16, cbg)

            def cbv(blk, ts, ps):
                st_i[1] += 1
                sv = stv[st_i[1] % 2]
                P.copy("act", sv, ps)
                P.dma("pool", self.vv[b][t0 + ts * 128:t0 + (ts + 1) * 128, blk * 512:(blk + 1) * 512], sv)
            self.linear_tm(a, n, R["wv"], D, cbv)

        self.proj_common(li, per_tile)
        P.barrier()

    def phase_attn_ret(self):
        P = self.P
        R = self.ret
        B = P.banks
        lg = P.tile("lg", [16])
        gnw = P.tile("gnw", [16])
        gnb = P.tile("gnb", [16])
        P.dma("sp", lg, R["decay_b"])
        P.dma("sp", gnw, R["gnw"])
        P.dma("sp", gnb, R["gnb"])
        P.act(lg, lg, AF.Sigmoid)
        P.act(lg, lg, AF.Ln)
        KT = P.tile("KT", [2, T])
        QT = P.tile("QT", [2, T])
        Vt = P.tile("Vt", [18, 256])
        e1 = [P.tile(f"e1_{i}", [512]) for i in range(2)]
        e2 = [P.tile(f"e2_{i}", [512]) for i in range(2)]
        pt = [P.tile(f"pt_{i}", [512]) for i in range(2)]
        df = [P.tile(f"df_{i}", [512]) for i in range(2)]
        db = [P.tile(f"db_{i}", [512]) for i in range(2)]
        osb = P.tile("osb", [2, 512])
        cen = P.tile("cen", [2, 512])
        sq = P.tile("sq", [2, 512])
        rs2 = P.tile("rs2", [2, 512])
        gt = P.tile("gt", [2, 512])
        yo = P.tile("yo", [2, 512])
        it = 0
        for b in range(self.nb):
            for h in range(8):
                P.dma("sp", KT, self.kT[b][h * 256:(h + 1) * 256, :].rr("(c p) t -> p c t", p=128))
                P.dma("sp", QT, self.qT[b][h * 256:(h + 1) * 256, :].rr("(c p) t -> p c t", p=128))
                P.dma("sp", Vt, self.vv[b][:, h * 256:(h + 1) * 256].rr("(st p) d -> p st d", p=128))
                for (t0, n) in TILES:
                    sts = [st for st in range(18) if not (st >= 2 and t0 < LCTX)]
                    O = [B[2], B[3]]
                    for i, st in enumerate(sts):
                        S = B[i % 2]
                        k = it % 2
                        it += 1
                        for c in range(2):
                            P.mm(S[:, 0:n], KT[:, c, st * 128:(st + 1) * 128], QT[:, c, t0:t0 + n], c == 0, c == 1)
                        P.dma("sp", df[k][:, 0:n], self.distf[st * 128:(st + 1) * 128, t0:t0 + n])
                        P.dma("sp", db[k][:, 0:n], self.distb[st * 128:(st + 1) * 128, t0:t0 + n])
                        P.act(e1[k][:, 0:n], df[k][:, 0:n], AF.Exp, scale=lg[:, h:h + 1])
                        P.act(e2[k][:, 0:n], db[k][:, 0:n], AF.Exp, scale=lg[:, 8 + h:9 + h])
                        P.tt("pool", e1[k][:, 0:n], e1[k][:, 0:n], e2[k][:, 0:n], ALU.add)
                        P.tt("dve", pt[k][:, 0:n], e1[k][:, 0:n], S[:, 0:n], ALU.mult)
                        for c in range(2):
                            P.mm(O[c][:, 0:n], Vt[:, st, c * 128:(c + 1) * 128], pt[k][:, 0:n], i == 0, i == len(sts) - 1)
                    self.head_norm_store(b, h * 2, 2, t0, n, O, osb, cen, sq, rs2, gt, yo, gnw, gnb, True, B[4], B[5])
        P.barrier()

    def head_norm_store(self, b, c0, nc_, t0, n, O, osb, cen, sq, rs2, gt, yo, gnw, gnb, center, bm, bv, src_sb=None):
        P = self.P
        dh = nc_ * 128
        if src_sb is None:
            for c in range(nc_):
                P.copy("act", osb[:, c, 0:n], O[c][:, 0:n])
        else:
            osb = src_sb
        if center:
            for c in range(nc_):
                P.mm32(bm[:, 0:n], self.ones, osb[:, c, 0:n], c == 0, c == nc_ - 1)
            for c in range(nc_):
                P.stt("dve", cen[:, c, 0:n], bm[:, 0:n], -1.0 / dh, osb[:, c, 0:n], ALU.mult, ALU.add)
        else:
            cen = osb
        P.act(sq[:, 0:nc_, 0:n], cen[:, 0:nc_, 0:n], AF.Square)
        for c in range(nc_):
            P.mm32(bv[:, 0:n], self.ones, sq[:, c, 0:n], c == 0, c == nc_ - 1)
        rstd = rs2[:, 0, 0:n]
        self.rsqrt(rstd, bv[:, 0:n], 1.0 / dh, rs2[:, 1, 0:n])
        P.dma("sp", gt[:, 0:nc_, 0:n], self.gT[b][c0 * 128:(c0 + nc_) * 128, t0:t0 + n].rr("(c p) t -> p c t", p=128))
        for c in range(nc_):
            P.stt("dve", yo[:, c, 0:n], cen[:, c, 0:n], gnw[:, c0 + c:c0 + c + 1], rstd, ALU.mult, ALU.mult)
            if gnb is not None:
                P.act(yo[:, c, 0:n], yo[:, c, 0:n], AF.Identity, bias=gnb[:, c0 + c:c0 + c + 1], scale=1.0)
            P.tt("pool", yo[:, c, 0:n], yo[:, c, 0:n], gt[:, c, 0:n], ALU.mult)
        P.dma("pool", self.oT[b][c0 * 128:(c0 + nc_) * 128, t0:t0 + n].rr("(c p) t -> p c t", p=128), yo[:, 0:nc_, 0:n])

    def setup_ml(self):
        self.ml = dict(
            wq=self.ext_in("ml_wq", [D, 1024]), wk=self.ext_in("ml_wk", [D, 1024]), wv=self.ext_in("ml_wv", [D, D]),
            wog=self.ext_in("ml_wog", [D, D]), wout=self.ext_in("ml_wout", [D, D]),
            wgi=self.ext_in("ml_wgi", [D, 16]), wgf=self.ext_in("ml_wgf", [D, 16]),
            bg=self.ext_in("ml_bg", [16, 2]), mlc=self.ext_in("ml_c", [16, 2]),
            selm=self.ext_in("ml_selm", [16, 16 * 128]), nw=self.ext_in("ml_norm_wT", [128, 16]))
        self.gates_d = self.P.dram("gates_d", [self.nb, 2, 16, T])

    def phase_proj_ml(self, li):
        P = self.P
        R = self.ml
        self.alloc_qkv(D)
        stg = [P.tile(f"stg{i}", [4, 512]) for i in range(2)]
        stv = [P.tile(f"stv{i}", [512]) for i in range(2)]
        wg = P.tile("wg", [2, 16, 16])
        bg = P.tile("bg", [2], nparts=16)
        grow = P.tile("grow", [2, 512], nparts=16)
        P.dma("sp", wg[:, 0], R["wgi"].rr("(kc p) e -> p kc e", p=128))
        P.dma("sp", wg[:, 1], R["wgf"].rr("(kc p) e -> p kc e", p=128))
        P.dma("sp", bg, R["bg"])
        st_i = [0, 0]

        def per_tile(b, ti, t0, n, a):
            for W, dst, scale in ((R["wq"], self.qT, 1.0), (R["wk"], self.kT, 128.0 ** -0.5)):
                def cb(oc, ps, dst=dst, scale=scale):
                    j = oc % 4
                    if j == 0:
                        st_i[0] += 1
                    stage = stg[st_i[0] % 2]
                    P.act(stage[:, j, 0:n], ps[:, 0:n], AF.Copy, scale=scale)
                    if j == 3:
                        self.store_fm(dst, b, oc - 3, 4, t0, n, stage)
                self.linear_fm(a, n, W, 8, cb)

            def cbg(oc, ps):
                j = oc % 4
                if j == 0:
                    st_i[0] += 1
                stage = stg[st_i[0] % 2]
                P.act(stage[:, j, 0:n], ps[:, 0:n], AF.Sigmoid)
                if j == 3:
                    self.store_fm(self.gT, b, oc - 3, 4, t0, n, stage)
            self.linear_fm(a, n, R["wog"], 16, cbg)

            def cbv(blk, ts, ps):
                st_i[1] += 1
                sv = stv[st_i[1] % 2]
                P.copy("act", sv, ps)
                P.dma("pool", self.vv[b][t0 + ts * 128:t0 + (ts + 1) * 128, blk * 512:(blk + 1) * 512], sv)
            self.linear_tm(a, n, R["wv"], D, cbv)
            for w_ in range(2):
                ps = P.bank()
                for kc in range(16):
                    P.mm32(ps[0:16, 0:n], wg[:, w_, kc, :], a[:, kc, 0:n], kc == 0, kc == 15)
                P.act(grow[:, w_, 0:n], ps[0:16, 0:n], AF.Identity, bias=bg[:, w_:w_ + 1], scale=1.0)
            P.dma("pool", self.gates_d[b][:, :, t0:t0 + n].rr("w r t -> r w t"), grow[:, :, 0:n])

        self.proj_common(li, per_tile)
        P.barrier()

    def phase_attn_ml(self):
        P = self.P
        R = self.ml
        B = P.banks
        df_h, db_h = dist_tables()
        dists = (self.distf, self.distb)

        def validity(dh, st, t0, n):
            blk = dh[st * 128:(st + 1) * 128, t0:t0 + n] < 1e8
            return 0 if not blk.any() else (2 if blk.all() else 1)

        nw = P.tile("nw", [16])
        P.dma("sp", nw, R["nw"])
        mlc = P.tile("mlc", [2], nparts=16)
        P.dma("sp", mlc, R["mlc"])
        selm = P.tile("selm", [16, 128], nparts=16)
        P.dma("sp", selm, R["selm"].rr("r (k m) -> r k m", m=128))
        Ir = P.tile("Ir", [T], nparts=16)
        Fr = P.tile("Fr", [T], nparts=16)
        pre = P.tile("pre", [T], nparts=16)
        Ph = P.tile("Ph", [T], nparts=16)
        onr = P.tile("onr", [T], nparts=16)
        gam = P.tile("gam", [2], nparts=16)
        icol = P.tile("icol", [18, 16])
        KT = P.tile("KT", [T])
        QT = P.tile("QT", [T])
        Vt = P.tile("Vt", [18, 256])
        phib = [P.tile(f"phib{i}", [512]) for i in range(2)]
        dtl = [P.tile(f"dtl{i}", [512]) for i in range(2)]
        wtl = [P.tile(f"wtl{i}", [512]) for i in range(2)]
        ptl = [P.tile(f"ptl{i}", [512]) for i in range(2)]
        dn = P.tile("dn", [2, 512])
        osb = P.tile("osb", [2, 512])
        hb_ = P.tile("hb_", [2, 512])
        cen = P.tile("cen", [2, 512])
        sq = P.tile("sq", [2, 512])
        rs2 = P.tile("rs2", [2, 512])
        gt = P.tile("gt", [2, 512])
        yo = P.tile("yo", [2, 512])
        P.memset("dve", onr, 1.0)
        alpha, beta = mlc[:, 0:1], mlc[:, 1:2]
        it = 0
        for b in range(self.nb):
            P.dma("sp", Ir, self.gates_d[b][0])
            P.dma("sp", Fr, self.gates_d[b][1])
            P.act(Fr, Fr, AF.Sigmoid)
            P.act(Fr, Fr, AF.Ln)
            for (c0, c1) in ((0, LCTX), (LCTX, T)):
                po, d0, d1 = pre.ap[:, c0:c1], onr.ap[:, c0:c1], Fr.ap[:, c0:c1]
                P.generic("dve", lambda e, po=po, d0=d0, d1=d1: e.tensor_tensor_scan(po, d0, d1, 0.0, ALU.mult, ALU.add),
                          [onr, Fr], [pre])
            P.ts("dve", gam[:, 0:1], pre[:, LCTX - 1:LCTX], beta, None, ALU.mult)
            P.stt("dve", gam[:, 1:2], pre[:, T - 1:T], beta, pre[:, LCTX - 1:LCTX], ALU.mult, ALU.add)
            P.ts("dve", Ph[:, 0:LCTX], pre[:, 0:LCTX], alpha, gam[:, 0:1], ALU.mult, ALU.add)
            P.ts("dve", Ph[:, LCTX:T], pre[:, LCTX:T], alpha, gam[:, 1:2], ALU.mult, ALU.add)
            P.stt("dve", Ph, Fr, beta, Ph, ALU.mult, ALU.add)
            P.tt("dve", Ir, Ir, Ph, ALU.subtract)
            for st in range(18):
                pT = B[st % 2]
                P.transpose(pT[:, 0:16], Ir[:, st * 128:(st + 1) * 128], self.ident[0:16, 0:16])
                P.copy("act", icol[:, st, :], pT[:, 0:16])
            for h in range(8):
                P.dma("sp", KT, self.kT[b][h * 128:(h + 1) * 128, :])
                P.dma("sp", QT, self.qT[b][h * 128:(h + 1) * 128, :])
                P.dma("sp", Vt, self.vv[b][:, h * 256:(h + 1) * 256].rr("(st p) d -> p st d", p=128))
                for (t0, n) in TILES:
                    for d in range(2):
                        P.mm32(B[d][:, 0:n], selm[:, d * 8 + h, :], Ph[:, t0:t0 + n], True, True)
                        P.copy("act", phib[d][:, 0:n], B[d][:, 0:n])
                    blocks = []
                    for st in range(18):
                        v = [validity(df_h, st, t0, n), validity(db_h, st, t0, n)]
                        if v[0] or v[1]:
                            blocks.append((st, v))
                    cnt = [sum(1 for _, v in blocks if v[d]) for d in range(2)]
                    seen = [0, 0]
                    O = [[B[2], B[3]], [B[5], B[6]]]
                    DEN = [B[4], B[7]]
                    for i, (st, v) in enumerate(blocks):
                        S = B[i % 2]
                        P.mm(S[:, 0:n], KT[:, st * 128:(st + 1) * 128], QT[:, t0:t0 + n], True, True)
                        for d in range(2):
                            if not v[d]:
                                continue
                            k = it % 2
                            it += 1
                            src = phib[d][:, 0:n]
                            if v[d] == 1:
                                P.dma("sp", dtl[k][:, 0:n], dists[d][st * 128:(st + 1) * 128, t0:t0 + n])
                                P.ts("dve", dtl[k][:, 0:n], dtl[k][:, 0:n], 1e8, -1e30, ALU.is_ge, ALU.mult)
                                P.tt("dve", dtl[k][:, 0:n], dtl[k][:, 0:n], phib[d][:, 0:n], ALU.add)
                                src = dtl[k][:, 0:n]
                            P.act(wtl[k][:, 0:n], src, AF.Exp, bias=icol[:, st, d * 8 + h:d * 8 + h + 1], scale=1.0)
                            P.tt("dve", ptl[k][:, 0:n], wtl[k][:, 0:n], S[:, 0:n], ALU.mult)
                            first, lastf = seen[d] == 0, seen[d] == cnt[d] - 1
                            seen[d] += 1
                            for c in range(2):
                                P.mm(O[d][c][:, 0:n], Vt[:, st, c * 128:(c + 1) * 128], ptl[k][:, 0:n], first, lastf)
                            P.mm(DEN[d][:, 0:n], self.ones, ptl[k][:, 0:n], first, lastf)
                    for d in range(2):
                        P.act(dn[:, d, 0:n], DEN[d][:, 0:n], AF.Abs)
                        P.ts("dve", dn[:, d, 0:n], dn[:, d, 0:n], 1.0, None, ALU.max)
                        oo, ii = dn.ap[:, d, 0:n], dn.ap[:, d, 0:n]
                        P.generic("dve", lambda e, oo=oo, ii=ii: e.reciprocal(oo, ii), [dn], [dn])
                    for c in range(2):
                        P.tt("dve", osb[:, c, 0:n], O[0][c][:, 0:n], dn[:, 0, 0:n], ALU.mult)
                        P.tt("dve", hb_[:, c, 0:n], O[1][c][:, 0:n], dn[:, 1, 0:n], ALU.mult)
                        P.tt("pool", osb[:, c, 0:n], osb[:, c, 0:n], hb_[:, c, 0:n], ALU.add)
                    self.head_norm_store(b, h * 2, 2, t0, n, None, osb, cen, sq, rs2, gt, yo, nw, None, True, B[0], B[1], src_sb=osb)
        P.barrier()

    def setup_na(self):
        self.na = dict(wqkv=self.ext_in("na_wqkv", [D, 3 * D]), wo=self.ext_in("na_wo", [D, D]),
                       tab=self.ext_in("na_tab", [8, 16, 512, 64]))

    def phase_proj_na(self, li):
        P = self.P
        R = self.na
        self.alloc_qkv(D)
        stg = [P.tile(f"stg{i}", [4, 512]) for i in range(2)]
        stv = [P.tile(f"stv{i}", [512]) for i in range(2)]
        st_i = [0, 0]

        def per_tile(b, ti, t0, n, a):
            for W, dst in ((R["wqkv"][:, 0:D], self.qT), (R["wqkv"][:, D:2 * D], self.kT)):
                def cb(oc, ps, dst=dst):
                    j = oc % 4
                    if j == 0:
                        st_i[0] += 1
                    stage = stg[st_i[0] % 2]
                    P.copy("act", stage[:, j, 0:n], ps[:, 0:n])
                    if j == 3:
                        self.store_fm(dst, b, oc - 3, 4, t0, n, stage)
                self.linear_fm(a, n, W, 16, cb)

            def cbv(blk, ts, ps):
                st_i[1] += 1
                sv = stv[st_i[1] % 2]
                P.copy("act", sv, ps)
                P.dma("pool", self.vv[b][t0 + ts * 128:t0 + (ts + 1) * 128, blk * 512:(blk + 1) * 512], sv)
            self.linear_tm(a, n, R["wqkv"][:, 2 * D:3 * D], D, cbv)

        self.proj_common(li, per_tile)
        P.barrier()

    def phase_attn_na(self):
        P = self.P
        R = self.na
        B = P.banks
        sc = 128.0 ** -0.5
        KT = P.tile("KT", [T])
        QT = P.tile("QT", [T])
        Vt = P.tile("Vt", [18, 128])
        Vt2 = P.tile("Vt2", [17, 128])
        tab = P.tile("tab", [8, 4, 64])
        pw = [P.tile(f"pw{i}", [6, 64]) for i in range(2)]
        tmp = [P.tile(f"tmp{i}", [4, 64]) for i in range(2)]
        psm = [P.tile(f"psm{i}", [64]) for i in range(2)]
        rec = [P.tile(f"rec{i}", [64]) for i in range(2)]
        stage = [P.tile(f"stage{i}", [512]) for i in range(2)]
        pc = P.tile("pc", [2, 256])
        recc = P.tile("recc", [256])
        oc_ = P.tile("oc_", [256])
        for b in range(self.nb):
            for h in range(16):
                P.dma("sp", KT, self.kT[b][h * 128:(h + 1) * 128, :])
                P.dma("sp", QT, self.qT[b][h * 128:(h + 1) * 128, :])
                P.dma("sp", Vt, self.vv[b][:, h * 128:(h + 1) * 128].rr("(st p) d -> p st d", p=128))
                P.dma("sp", Vt2, self.vv[b][64:64 + 17 * 128, h * 128:(h + 1) * 128].rr("(st p) d -> p st d", p=128))
                for t_ in range(8):
                    P.dma("sp", tab[:, t_], R["tab"][t_, h].rr("(j p) q -> p j q", p=128))
                S = B[0]
                Sv = V(S.ap.rearrange("p (j q) -> p j q", q=256), S.buf)
                for st in range(2):
                    P.mm(Sv[:, st, :], KT[:, st * 128:(st + 1) * 128], QT[:, 0:LCTX], True, True)
                P.act(pc, Sv, AF.Exp, scale=sc)
                O, DEN = B[1], B[2]
                for st in range(2):
                    P.mm(O[:, 0:LCTX], Vt[:, st, :], pc[:, st, :], st == 0, st == 1)
                for st in range(2):
                    P.mm(DEN[:, 0:LCTX], self.ones, pc[:, st, :], st == 0, st == 1)
                ro, ri = recc.ap, DEN.ap[:, 0:LCTX]
                P.generic("dve", lambda e, ro=ro, ri=ri: e.reciprocal(ro, ri), [DEN], [recc])
                P.tt("dve", oc_, O[:, 0:LCTX], recc, ALU.mult)
                P.dma("pool", self.oT[b][h * 128:(h + 1) * 128, 0:LCTX], oc_)
                import os
                for r in range(int(os.environ.get("NA_ROWS", "32"))):
                    k = r % 2
                    r0 = min(max(r - 4, 0), 24)
                    rt = r if r <= 3 else (4 if r <= 28 else r - 24)
                    q0 = LCTX + r * 64
                    S = B[3 + (r % 2)]
                    Sv = V(S.ap[:, 0:384].rearrange("p (j q) -> p j q", q=64), S.buf)
                    kbase = LCTX + r0 * 64
                    vts = [Vt[:, 0, :], Vt[:, 1, :]]
                    for j in range(4):
                        if r0 % 2 == 0:
                            vts.append(Vt[:, 2 + r0 // 2 + j, :])
                        else:
                            vts.append(Vt2[:, (192 + r0 * 64) // 128 + j, :])
                    for j in range(6):
                        ks = j * 128 if j < 2 else kbase + (j - 2) * 128
                        P.mm(Sv[:, j, :], KT[:, ks:ks + 128], QT[:, q0:q0 + 64], True, True)
                    P.act(pw[k][:, 0:2, :], Sv[:, 0:2, :], AF.Exp, scale=sc)
                    P.stt("dve", tmp[k], Sv[:, 2:6, :], sc, tab[:, rt], ALU.mult, ALU.add)
                    P.act(pw[k][:, 2:6, :], tmp[k], AF.Exp)
                    O, DEN = B[5 + (r % 2)], B[7 if r % 2 else 1]
                    for j in range(6):
                        P.mm(O[:, 0:64], vts[j], pw[k][:, j, :], j == 0, j == 5)
                    for j in range(6):
                        P.mm(DEN[:, 0:64], self.ones, pw[k][:, j, :], j == 0, j == 5)
                    ro, ri = rec[k].ap, DEN.ap[:, 0:64]
                    P.generic("dve", lambda e, ro=ro, ri=ri: e.reciprocal(ro, ri), [DEN], [rec[k]])
                    stg_ = stage[(r // 8) % 2]
                    P.tt("dve", stg_[:, (r % 8) * 64:(r % 8 + 1) * 64], O[:, 0:64], rec[k], ALU.mult)
                    if r % 8 == 7:
                        P.dma("pool", self.oT[b][h * 128:(h + 1) * 128, LCTX + (r - 7) * 64:LCTX + (r + 1) * 64], stg_)
        P.barrier()

    def setup_hg(self):
        nb = self.nb
        self.hg = dict(wq=self.ext_in("hg_wq", [D, D]), wi=self.ext_in("hg_wi", [D, D]),
                       wf=self.ext_in("hg_wf", [2, D, D]), wg=self.ext_in("hg_wg", [D, D]),
                       wo=self.ext_in("hg_wo", [D, D]), nw=self.ext_in("hg_norm_wT", [128, 16]),
                       lb=self.ext_in("hg_lb", [4, D]), tri=self.ext_in("hg_tri", [4, 128, 128]))
        self.kk_d = self.P.dram("kk_d", [nb, 2, T, D])
        self.gl_d = self.P.dram("gl_d", [nb, 2, T, D])

    def phase_proj_hg(self, li):
        P = self.P
        R = self.hg
        self.alloc_qkv(D)
        stg = [P.tile(f"stg{i}", [4, 512]) for i in range(2)]
        stv = [P.tile(f"stv{i}", [512]) for i in range(6)]
        oml = P.tile("oml", [D])
        st_i = [0, 0]
        wt0 = P.tile("wt0", [16, 512])
        wt1 = P.tile("wt1", [16, 512])
        Ev = V(wt0.ap.rearrange("p a b -> p (a b)").rearrange("p (l d) -> p l d", l=4), wt0.buf)
        for l_ in range(4):
            P.dma("sp", Ev[:, l_, :], V(R["lb"].ap[l_:l_ + 1, :].partition_broadcast(128).rearrange("p a n -> p (a n)"), None))
        P.act(Ev, Ev, AF.Exp)
        P.tt("dve", oml, Ev[:, 0, :], Ev[:, 1, :], ALU.add)
        P.tt("dve", oml, oml, Ev[:, 2, :], ALU.add)
        P.tt("dve", oml, oml, Ev[:, 3, :], ALU.add)
        oo = oml.ap
        P.generic("dve", lambda e: e.reciprocal(oo, oo), [oml], [oml])
        P.tt("dve", oml, oml, Ev[:, 0, :], ALU.mult)

        def per_tile(b, ti, t0, n, a):
            for W, dst, fn, scale in ((R["wq"], self.qT, AF.Silu, 128.0 ** -0.5), (R["wg"], self.gT, AF.Silu, 1.0)):
                def cb(oc, ps, dst=dst, fn=fn, scale=scale):
                    j = oc % 4
                    if j == 0:
                        st_i[0] += 1
                    stage = stg[st_i[0] % 2]
                    P.act(stage[:, j, 0:n], ps[:, 0:n], fn)
                    if scale != 1.0:
                        P.ts("pool", stage[:, j, 0:n], stage[:, j, 0:n], scale, None, ALU.mult)
                    if j == 3:
                        self.store_fm(dst, b, oc - 3, 4, t0, n, stage)
                self.linear_fm(a, n, W, 16, cb)

            def cbv(blk, ts, ps):
                st_i[1] += 1
                sv = stv[st_i[1] % 6]
                P.copy("act", sv, ps)
                P.dma("pool", self.vv[b][t0 + ts * 128:t0 + (ts + 1) * 128, blk * 512:(blk + 1) * 512], sv)
            self.linear_tm(a, n, R["wi"], D, cbv)
            for d in range(2):
                def cbz(blk, ts, ps, d=d):
                    st_i[1] += 1
                    kt = stv[st_i[1] % 6]
                    st_i[1] += 1
                    gl = stv[st_i[1] % 6]
                    P.act(kt, ps, AF.Sigmoid, scale=-1.0)
                    P.tt("dve", kt, kt, oml[:, blk * 512:(blk + 1) * 512], ALU.mult)
                    P.dma("pool", self.kk_d[b, d][t0 + ts * 128:t0 + (ts + 1) * 128, blk * 512:(blk + 1) * 512], kt)
                    P.act(gl, kt, AF.Ln, bias=self.ones[:, 0:1], scale=-1.0)
                    P.dma("pool", self.gl_d[b, d][t0 + ts * 128:t0 + (ts + 1) * 128, blk * 512:(blk + 1) * 512], gl)
                self.linear_tm(a, n, R["wf"][d], D, cbz)

        self.wt = [wt0, wt1]
        self.wt_i = 0
        h = P.tile("h", [16, 512])
        a = P.tile("a", [16, 512])
        rs = P.tile("rs", [2, 512])
        src = self.hT0 if li == self.layers[0] else self.hT
        for b in range(self.nb):
            for ti, (t0, n) in enumerate(TILES):
                P.dma("sp", h[:, :, 0:n], src[b][:, t0:t0 + n].rr("(kc p) t -> p kc t", p=128))
                j = 2 if ti == 0 else b
                self.norm_mod(h, a, n, 0, j, rs)
                per_tile(b, ti, t0, n, a)
        P.barrier()

    def phase_attn_hg(self):
        P = self.P
        R = self.hg
        B = P.banks
        nw = P.tile("nw", [16])
        P.dma("sp", nw, R["nw"])
        tri = P.tile("tri", [4, 128])
        for i in range(4):
            P.dma("sp", tri[:, i, :], R["tri"][i])
        U, Ls, L, Us = tri[:, 0, :], tri[:, 1, :], tri[:, 2, :], tri[:, 3, :]
        QT = P.tile("QT", [T])
        Vt = P.tile("Vt", [18, 128])
        Kt = [P.tile(f"Kt{d}", [18, 128]) for d in range(2)]
        Gt = [P.tile(f"Gt{d}", [18, 128]) for d in range(2)]
        Oa = [P.tile(f"Oa{d}", [T]) for d in range(2)]
        Sst = [P.tile(f"S{d}", [128]) for d in range(2)]
        names = ("ebT", "enb", "eE", "dec", "qin", "kin", "kinT", "kend", "Am")
        tl = [{nm: P.tile(f"{nm}{d}", [128] if nm != "dec" else [1]) for nm in names} for d in range(2)]
        sq = P.tile("sq", [1, 512])
        rs2 = P.tile("rs2", [2, 512])
        gt = P.tile("gt", [1, 512])
        yo = P.tile("yo", [1, 512])
        orders = ([0, 1] + list(range(2, 18)), [1, 0] + list(range(17, 1, -1)))
        for b in range(self.nb):
            for h in range(16):
                P.dma("sp", QT, self.qT[b][h * 128:(h + 1) * 128, :])
                P.dma("sp", Vt, self.vv[b][:, h * 128:(h + 1) * 128].rr("(st p) d -> p st d", p=128))
                for d in range(2):
                    P.dma("sp", Kt[d], self.kk_d[b, d][:, h * 128:(h + 1) * 128].rr("(st p) d -> p st d", p=128))
                    P.dma("sp", Gt[d], self.gl_d[b, d][:, h * 128:(h + 1) * 128].rr("(st p) d -> p st d", p=128))
                for step in range(18):
                    for d in range(2):
                        c = orders[d][step]
                        tt_ = tl[d]
                        X0, X1, X2 = B[3 * d], B[3 * d + 1], B[3 * d + 2]
                        Mi, Me, Mk = (U, Ls, U) if d == 0 else (L, Us, L)
                        g_c, k_c, v_c = Gt[d][:, c, :], Kt[d][:, c, :], Vt[:, c, :]
                        cs = slice(c * 128, (c + 1) * 128)
                        bps, bTps, Eps, kTps = X0[:, 0:128], X0[:, 128:256], X0[:, 256:384], X0[:, 384:512]
                        P.mm32(bps, Mi, g_c, True, True)
                        P.mm32(bTps, g_c, Mi, True, True)
                        P.mm32(Eps, Me, g_c, True, True)
                        P.act(tt_["enb"], bps, AF.Exp, scale=-1.0)
                        P.act(tt_["ebT"], bTps, AF.Exp)
                        P.act(tt_["eE"], Eps, AF.Exp)
                        lastcol = bTps[:, 127:128] if d == 0 else bTps[:, 0:1]
                        P.act(tt_["dec"], lastcol, AF.Exp)
                        P.tt("dve", tt_["kin"], k_c, tt_["enb"], ALU.mult)
                        P.tt("pool", tt_["qin"], QT[:, cs], tt_["ebT"], ALU.mult)
                        P.tt("pool", tt_["kend"], k_c, tt_["eE"], ALU.mult)
                        P.transpose(kTps, tt_["kin"], self.ident)
                        P.copy("act", tt_["kinT"], kTps)
                        Aps, Sps = X1[:, 0:128], X1[:, 128:256]
                        P.mm32(Aps, tt_["kinT"], tt_["qin"], True, True)
                        P.tt("dve", tt_["Am"], Aps, Mk, ALU.mult)
                        ops_ = X2[:, 0:128]
                        P.mm32(ops_, v_c, tt_["Am"], True, step == 0)
                        if step > 0:
                            P.mm32(ops_, Sst[d], tt_["qin"], False, True)
                        P.copy("act", Oa[d][:, cs], ops_)
                        P.mm32(Sps, tt_["kend"], v_c, True, True)
                        if step == 0:
                            P.copy("dve", Sst[d], Sps)
                        else:
                            P.stt("dve", Sst[d], Sst[d], tt_["dec"][:, 0:1], Sps, ALU.mult, ALU.add)
                P.tt("dve", Oa[0], Oa[0], Oa[1], ALU.add)
                for (t0, n) in TILES:
                    osb = V(Oa[0].ap[:, t0:t0 + n].unsqueeze(1), Oa[0].buf)
                    self.head_norm_store(b, h, 1, t0, n, None, osb, None, sq, rs2, gt, yo, nw, None, False, B[6], B[7], src_sb=osb)
        P.barrier()

    def phase_out(self, idx, li, Wo, last):
        P = self.P
        self.wt = [P.tile(f"wt{i}", [16, 512]) for i in range(2)]
        self.wt_i = 0
        o = P.tile("o", [16, 512])
        h = P.tile("h", [16, 512])
        f = P.tile("f", [16, 512])
        rs = P.tile("rs", [2, 512])
        rw = P.tile("rw", [16, 16])
        lgt = P.tile("lgt", [16])
        ssum = P.tile("ssum", [1])
        P.dma("sp", rw, self.router[idx].rr("(kc p) e -> p kc e", p=128))
        src = self.hT0 if li == self.layers[0] else self.hT
        for b in range(self.nb):
            for ti, (t0, n) in enumerate(TILES):
                if last and ti == 0:
                    continue
                j = 2 if ti == 0 else b
                P.dma("sp", o[:, :, 0:n], self.oT[b][:, t0:t0 + n].rr("(kc p) t -> p kc t", p=128))
                P.dma("sp", h[:, :, 0:n], src[b][:, t0:t0 + n].rr("(kc p) t -> p kc t", p=128))

                def cb(oc, ps):
                    P.stt("dve", h[:, oc, 0:n], ps[:, 0:n], self.modv(2, oc, j), h[:, oc, 0:n], ALU.mult, ALU.add)
                self.linear_fm(o, n, Wo, 16, cb)
                P.dma("pool", self.hT[b][:, t0:t0 + n].rr("(kc p) t -> p kc t", p=128), h[:, :, 0:n])
                self.norm_mod(h, f, n, 1, j, rs)
                P.dma("pool", self.fT[b][:, t0:t0 + n].rr("(kc p) t -> p kc t", p=128), f[:, :, 0:n])
                for ts in range(n // 128):
                    ps = P.bank()
                    for kc in range(16):
                        P.mm32(ps[:, 0:16], f[:, kc, ts * 128:(ts + 1) * 128], rw[:, kc, :], kc == 0, kc == 15)
                    P.act(lgt, ps[:, 0:16], AF.Exp, accum=ssum)
                    oo, ii = ssum.ap, ssum.ap
                    P.generic("dve", lambda e, oo=oo, ii=ii: e.reciprocal(oo, ii), [ssum], [ssum])
                    P.ts("dve", lgt, lgt, ssum[:, 0:1], None, ALU.mult)
                    pT = P.bank()
                    P.transpose(pT[0:16, 0:128], lgt, self.ident)
                    P.copy("act", self.affT[b][:, t0 + ts * 128:t0 + (ts + 1) * 128], pT[0:16, 0:128])
        P.barrier()

    def phase_topk(self, last):
        P = self.P
        wk = P.tile("wk", [NLAT], nparts=16)
        m8 = P.tile("m8", [8], nparts=16)
        G = P.tile("G", [T], nparts=16)
        for b in range(self.nb):
            for (c0, ncol, rounds) in ((LCTX, NLAT, 32), (0, LCTX, 4)):
                if last and c0 == 0:
                    continue
                aff = self.affT[b][:, c0:c0 + ncol]
                w = wk[:, 0:ncol]
                P.copy("dve", w, aff)
                for r in range(rounds):
                    mo, wi = m8.ap, w.ap
                    P.generic("dve", lambda e, mo=mo, wi=wi: e.max(mo, wi), [w], [m8])
                    if r < rounds - 1:
                        P.generic("dve", lambda e, mo=mo, wi=wi: e.match_replace(wi, mo, wi, -1.0), [w, m8], [w])
                P.stt("dve", G[:, c0:c0 + ncol], aff, m8[:, 7:8], aff, ALU.is_ge, ALU.mult)
            if last:
                P.dma("pool", self.Gd[b][:, LCTX:T], G[:, LCTX:T])
            else:
                P.dma("pool", self.Gd[b], G)
        P.barrier()

    def phase_moe(self, idx, last):
        P = self.P
        self.wt = [P.tile(f"wt{i}", [16, 512]) for i in range(2)]
        self.wt_i = 0
        f = P.tile("f", [16, 512])
        acc = P.tile("acc", [16, 512])
        hm = P.tile("hm", [8, 512])
        gbt = [P.tile(f"gbt{i}", [512]) for i in range(2)]
        su = [P.tile(f"su{i}", [512]) for i in range(2)]
        rs = P.tile("rs", [2, 512])
        k = 0
        for b in range(self.nb):
            for ti, (t0, n) in enumerate(TILES):
                if last and ti == 0:
                    continue
                j = 2 if ti == 0 else b
                P.dma("sp", f[:, :, 0:n], self.fT[b][:, t0:t0 + n].rr("(kc p) t -> p kc t", p=128))
                for e in range(16):
                    gb = gbt[e % 2]
                    P.dma("sp", gb[:, 0:n], V(self.Gd.ap[b, e:e + 1, t0:t0 + n].partition_broadcast(128).rearrange("p a n -> p (a n)"), self.Gd.buf))
                    for half in range(2):
                        w1t = self.wtile()
                        P.dma("sp", w1t, self.w1[idx, e][:, half * 512:(half + 1) * 512].rr("(kc p) n -> p kc n", p=128))
                        w3t = self.wtile()
                        P.dma("sp", w3t, self.w3[idx, e][:, half * 512:(half + 1) * 512].rr("(kc p) n -> p kc n", p=128))
                        for jj in range(4):
                            fc = half * 4 + jj
                            psu = P.bank()
                            for kc in range(16):
                                P.mm(psu[:, 0:n], w1t[:, kc, jj * 128:(jj + 1) * 128], f[:, kc, 0:n], kc == 0, kc == 15)
                            psg = P.bank()
                            for kc in range(16):
                                P.mm(psg[:, 0:n], w3t[:, kc, jj * 128:(jj + 1) * 128], f[:, kc, 0:n], kc == 0, kc == 15)
                            s_ = su[k % 2]
                            k += 1
                            P.act(s_[:, 0:n], psu[:, 0:n], AF.Silu)
                            P.tt("dve", hm[:, fc, 0:n], s_[:, 0:n], psg[:, 0:n], ALU.mult)
                            P.tt("pool", hm[:, fc, 0:n], hm[:, fc, 0:n], gb[:, 0:n], ALU.mult)
                    for blk in range(4):
                        w2t = self.wtile()
                        P.dma("sp", w2t[:, 0:8, :], self.w2[idx, e][:, blk * 512:(blk + 1) * 512].rr("(kc p) n -> p kc n", p=128))
                        for jj in range(4):
                            oc = blk * 4 + jj
                            psy = P.bank()
                            for fc in range(8):
                                P.mm(psy[:, 0:n], w2t[:, fc, jj * 128:(jj + 1) * 128], hm[:, fc, 0:n], fc == 0, fc == 7)
                            if e == 0:
                                P.copy("act", acc[:, oc, 0:n], psy[:, 0:n])
                            else:
                                P.tt("dve", acc[:, oc, 0:n], acc[:, oc, 0:n], psy[:, 0:n], ALU.add)
                P.dma("sp", f[:, :, 0:n], self.hT[b][:, t0:t0 + n].rr("(kc p) t -> p kc t", p=128))
                for oc in range(16):
                    P.stt("dve", f[:, oc, 0:n], acc[:, oc, 0:n], self.modv(5, oc, j), f[:, oc, 0:n], ALU.mult, ALU.add)
                if not last:
                    P.dma("pool", self.hT[b][:, t0:t0 + n].rr("(kc p) t -> p kc t", p=128), f[:, :, 0:n])
                else:
                    self.norm_mod(f, acc, n, 0, 0, rs, plain_g=self.fg)
                    P.dma("pool", self.outT[b][:, t0 - LCTX:t0 - LCTX + n].rr("(kc p) t -> p kc t", p=128), acc[:, :, 0:n], final=True)
        P.barrier()

    def emit_all(self):
        P = self.P
        self.setup_inputs_all()
        self.setup()
        nl = len(self.layers)
        for idx, li in enumerate(self.layers):
            kind = li % 4
            last = li == 3
            self.phase_mods(idx)
            if self.stop == "mods":
                self.dump("mod", self.mod_dump())
                return
            if kind == 0:
                self.phase_proj_ret(li)
                if self.stop == "proj":
                    for nm in ("qT", "kT", "gT", "vv"):
                        self.dump(nm, getattr(self, nm))
                    return
                self.phase_attn_ret()
                Wo = self.ret["wo"]
            elif kind == 1:
                self.phase_proj_ml(li)
                if self.stop == "proj":
                    for nm in ("qT", "kT", "gT", "vv", "gates_d"):
                        self.dump(nm, getattr(self, nm))
                    return
                self.phase_attn_ml()
                Wo = self.ml["wout"]
            elif kind == 3:
                self.phase_proj_hg(li)
                if self.stop == "proj":
                    for nm in ("qT", "gT", "vv", "kk_d", "gl_d"):
                        self.dump(nm, getattr(self, nm))
                    return
                self.phase_attn_hg()
                Wo = self.hg["wo"]
            elif kind == 2:
                self.phase_proj_na(li)
                if self.stop == "proj":
                    for nm in ("qT", "kT", "vv"):
                        self.dump(nm, getattr(self, nm))
                    return
                self.phase_attn_na()
                Wo = self.na["wo"]
            if self.stop == "attn":
                self.dump("oT", self.oT)
                return
            self.phase_out(idx, li, Wo, last)
            if self.stop == "out":
                self.dump("hT", self.hT)
                self.dump("fT", self.fT)
                return
            self.phase_topk(last)
            if self.stop == "topk":
                self.dump("Gd", self.Gd)
                return
            self.phase_moe(idx, last)
            if self.stop == f"layer{li}":
                self.dump("hT", self.hT)
                return

    def mod_dump(self):
        d = self.P.dram("mod_d", [128, 96 * 4])
        self.P.dma("sp", d, self.mod.rr("p a b -> p (a b)"))
        return d

    def setup_inputs_all(self):
        kinds = set(li % 4 for li in self.layers)
        if 0 in kinds:
            self.setup_ret()
        if 1 in kinds:
            self.setup_ml()
        if 2 in kinds:
            self.setup_na()
        if 3 in kinds:
            self.setup_hg()

    def dump(self, name, view):
        o = self.ext_out("dbg_" + name, view.shape)
        self.P.dma("sp", o, view, final=True)


def colform(v):
    v = np.asarray(v, np.float32)
    return np.ascontiguousarray(v.reshape(-1, 128).T)


def rope_tables():
    t = np.arange(NLAT)
    row = (t // 64).astype(np.float32)
    col = (t % 64).astype(np.float32)
    quarter = 64
    inv = (np.float32(10000.0) ** (-np.arange(quarter, dtype=np.float32) / quarter)).astype(np.float32)
    ang = np.concatenate([row[:, None] * inv, col[:, None] * inv], -1).astype(np.float32)
    return np.ascontiguousarray(np.cos(ang).T.astype(np.float32)), np.ascontiguousarray(np.sin(ang).T.astype(np.float32))


def dist_tables():
    idx = np.arange(T)
    is_ctx = idx < LCTX
    tau_f = idx.astype(np.float64)
    tau_b = np.where(is_ctx, LCTX - 1 - idx, LCTX + NLAT - 1 - (idx - LCTX)).astype(np.float64)
    out = []
    for tau in (tau_f, tau_b):
        d = tau[None, :] - tau[:, None]
        ok = d >= 0
        ok &= ~(is_ctx[None, :] & ~is_ctx[:, None])
        out.append(np.where(ok, d, 1e9).astype(np.float32))
    return out


def na_bias_table(rpb):
    rpb = np.asarray(rpb, np.float32)
    tab = np.full((8, 16, 512, 64), -1e30, np.float32)
    rows_for_type = [0, 1, 2, 3, 10, 29, 30, 31]
    qc = np.arange(64)
    c0 = np.clip(qc - 8, 0, 48)
    for rt, r in enumerate(rows_for_type):
        r0 = min(max(r - 4, 0), 24)
        for wr in range(8):
            dr = r0 + wr - r
            kc = np.arange(64)
            dc = kc[:, None] - qc[None, :]
            ok = (kc[:, None] >= c0[None, :]) & (kc[:, None] < c0[None, :] + 16)
            dcc = np.clip(dc, -15, 15)
            vals = rpb[:, dr + 7, :][:, dcc + 15]
            blk = tab[rt, :, wr * 64:(wr + 1) * 64, :]
            blk[:, ok] = vals[:, ok]
    return tab


def prep_core_inputs(inp, core, layers, names, nb=NB):
    b0 = core * nb
    out = {}
    L = list(layers)
    for name in names:
        if name == "hT0":
            if "_h_override" in inp:
                out[name] = inp["_h_override"]
            else:
                out[name] = np.ascontiguousarray(np.concatenate(
                    [inp["ctx"][b0:b0 + nb].transpose(0, 2, 1), inp["x"][b0:b0 + nb].transpose(0, 2, 1)], axis=2))
        elif name == "cvec":
            cv = np.zeros((128, 16, 4), np.float32)
            for j in range(nb):
                cv[:, :, j] = colform(inp["c"][b0 + j])
            cv[:, :, 2] = colform(inp["c_ctx"])
            out[name] = cv
        elif name == "ada_w":
            out[name] = np.ascontiguousarray(inp["ada_w"][L])
        elif name == "ada_bT":
            out[name] = np.stack([colform(inp["ada_b"][i]) for i in L])
        elif name == "normgT":
            out[name] = np.stack([np.stack([colform(inp["norm_g"][i, w]) for w in range(2)]) for i in L])
        elif name == "final_gT":
            out[name] = colform(inp["final_g"])
        elif name == "router":
            out[name] = np.ascontiguousarray(inp["moe_router"][L])
        elif name in ("moe_w1", "moe_w3", "moe_w2"):
            out[name] = np.ascontiguousarray(inp[name][L])
        elif name == "ident":
            out[name] = np.eye(128, dtype=np.float32)
        elif name == "ret_decay_b":
            out[name] = np.ascontiguousarray(np.broadcast_to(inp["ret_decay"].reshape(1, 16), (128, 16)))
        elif name in ("ret_gn_wT", "ret_gn_bT", "ml_norm_wT", "hg_norm_wT"):
            out[name] = colform(inp[name[:-1]])
        elif name == "rope_cosT":
            out[name] = rope_tables()[0]
        elif name == "rope_sinT":
            out[name] = rope_tables()[1]
        elif name == "dist_f":
            out[name] = dist_tables()[0]
        elif name == "dist_b":
            out[name] = dist_tables()[1]
        elif name == "hg_tri":
            i_ = np.arange(128)
            sm, tm = i_[:, None], i_[None, :]
            out[name] = np.stack([(sm <= tm), (sm > tm), (sm >= tm), (sm < tm)]).astype(np.float32)
        elif name == "na_tab":
            out[name] = na_bias_table(inp["na_rpb"])
        elif name == "ml_wgi":
            out[name] = np.ascontiguousarray(np.concatenate([inp["ml_wgate"][0][:, :8], inp["ml_wgate"][1][:, :8]], 1))
        elif name == "ml_wgf":
            out[name] = np.ascontiguousarray(np.concatenate([inp["ml_wgate"][0][:, 8:], inp["ml_wgate"][1][:, 8:]], 1))
        elif name == "ml_bg":
            bgt = inp["ml_bgate"]
            out[name] = np.ascontiguousarray(np.stack([np.concatenate([bgt[0][:8], bgt[1][:8]]),
                                                       np.concatenate([bgt[0][8:], bgt[1][8:]])], 1))
        elif name == "ml_c":
            mc = np.zeros((16, 2), np.float32)
            mc[:8, 0] = 1.0
            mc[8:, 0] = -1.0
            mc[8:, 1] = 1.0
            out[name] = mc
        elif name == "ml_selm":
            sm = np.zeros((16, 16, 128), np.float32)
            for r_ in range(16):
                sm[r_, r_, :] = 1.0
            out[name] = sm.reshape(16, 16 * 128)
        elif name in inp:
            out[name] = np.ascontiguousarray(inp[name])
        else:
            raise KeyError(name)
    return out


def build_program(layers, nb=NB, dbg=None, stop=None):
    nc = bass.Bass("TRN2", target_bir_lowering=False)
    es = ExitStack()
    P = Prog(nc, es)
    M = Model(P, layers, dbg, nb)
    M.stop = stop
    M.emit_all()
    P.barrier()
    P.finalize(es)
    es.close()
    return nc, M


def kernel(**inputs):
    inp = {k: np.asarray(v) for k, v in inputs.items()}
    per_core = ("hT0", "cvec")
    h_cur = None
    out = None
    for L in range(4):
        layers = [L]
        nc, M = build_program(layers)
        names = list(M.inputs.keys())
        shared = prep_core_inputs(inp, 0, layers, [n for n in names if n not in per_core])
        in_maps = []
        for c in range(NCORES):
            m = dict(shared)
            pc = prep_core_inputs(inp, c, layers, ["cvec"] + (["hT0"] if h_cur is None else []))
            if h_cur is not None:
                pc["hT0"] = h_cur[c]
            m.update(pc)
            in_maps.append(m)
        res = run_bass_kernel_spmd(nc, in_maps, core_ids=list(range(NCORES)))
        if L < 3:
            h_cur = [res.results[c]["hT_out"] for c in range(NCORES)]
        else:
            out = np.empty((NCORES * NB, NLAT, D), np.float32)
            for c in range(NCORES):
                out[c * NB:(c + 1) * NB] = res.results[c]["outT"].transpose(0, 2, 1)
        del res, in_maps, shared
    return out
```

```python
import numpy as np
from contextlib import ExitStack
import concourse.bass as bass
import concourse.mybir as mybir
from concourse.bass_utils import run_bass_kernel_spmd

F32 = mybir.dt.float32
F32R = mybir.dt.float32r
AF = mybir.ActivationFunctionType
ALU = mybir.AluOpType
AX = mybir.AxisListType

D = 2048
NCH = 16
LCTX = 256
NLAT = 2048
T = LCTX + NLAT
NB = 2
NCORES = 8
TILES = [(0, 256), (256, 512), (768, 512), (1280, 512), (1792, 512)]
EPS = 1e-6
SAME_SYNC = True
RING = 12
import os
FAST = os.environ.get('K_FAST', '0') == '1'


class Buf:
    __slots__ = ("name", "multi", "writers", "readers", "psum")

    def __init__(self, name, multi=False):
        self.name = name
        self.multi = multi
        self.psum = name.startswith("bank")
        self.writers = []
        self.readers = []


class V:
    __slots__ = ("ap", "buf")

    def __init__(self, ap, buf):
        self.ap = ap
        self.buf = buf

    def __getitem__(self, k):
        return V(self.ap[k], self.buf)

    def rr(self, pat, **kw):
        return V(self.ap.rearrange(pat, **kw), self.buf)

    def bc(self, shape):
        return V(self.ap.to_broadcast(list(shape)), self.buf)

    def un(self, axis):
        return V(self.ap.unsqueeze(axis), self.buf)

    def wb(self, buf):
        return V(self.ap, buf)

    @property
    def shape(self):
        return self.ap.shape


class Op:
    __slots__ = ("eng", "fn", "deps", "is_dma", "ms", "sem", "val", "prev")


def _mmap(v):
    if FAST:
        return v.ap.bitcast(F32R)
    return v.ap


class Prog:
    ENGS = ("sp", "act", "dve", "pool", "pe")

    def __init__(self, nc, es):
        self.nc = nc
        self.ops = {e: [] for e in self.ENGS}
        self.bufs = []
        self.dma_since = []
        self.final = []
        self.sb = es.enter_context(nc.sbuf_tensor("sb", [128, 49152], F32))
        self.ps = es.enter_context(nc.psum_tensor("ps", [128, 4096], F32))
        self.sb_top = 0
        self.sb_base = 0
        self.sb_ptr = 0
        self.banks = [V(self.ps[:, i * 512:(i + 1) * 512], self.newbuf(f"bank{i}")) for i in range(8)]
        self.bank_rr = 0
        self.nops = 0

    def newbuf(self, name, multi=False):
        b = Buf(name, multi)
        self.bufs.append(b)
        return b

    def _alloc(self, shape, persistent):
        n = int(np.prod(shape))
        if persistent:
            assert self.sb_ptr == self.sb_base, "persistent alloc only between phases"
            off = self.sb_base
            self.sb_base += n
            self.sb_ptr = self.sb_base
        else:
            off = self.sb_ptr
            self.sb_ptr += n
        assert self.sb_ptr <= 49152, f"SBUF overflow {self.sb_ptr}"
        ap = self.sb[:, off:off + n]
        if len(shape) == 2:
            ap = ap.rearrange("p (a b) -> p a b", b=shape[1])
        elif len(shape) == 3:
            ap = ap.rearrange("p (a b c) -> p a b c", b=shape[1], c=shape[2])
        return ap

    def tile(self, name, shape, persistent=False, nparts=128):
        ap = self._alloc(shape, persistent)
        if nparts != 128:
            ap = ap[0:nparts]
        return V(ap, self.newbuf(name))

    def bank(self):
        b = self.banks[self.bank_rr % 8]
        self.bank_rr += 1
        return b

    def dram(self, name, shape, kind="Internal", tracked=True, multi=True):
        ap = self.nc.dram_tensor(name, list(shape), F32, kind=kind).ap()
        return V(ap, self.newbuf(name, multi=multi) if tracked else None)

    def _add(self, eng, fn, reads, writes, is_dma=False):
        op = Op()
        op.eng = eng
        op.fn = fn
        op.is_dma = is_dma
        op.ms = False
        op.sem = None
        op.val = 0
        op.prev = 0
        deps = []
        wb = []
        for v in writes:
            b = v.buf
            if b is None or b in wb:
                continue
            wb.append(b)
        rb = []
        for v in reads:
            b = v.buf
            if b is None or b in wb or b in rb:
                continue
            rb.append(b)
        for b in rb:
            deps.extend(b.writers)
            if b.psum:
                deps.extend(r_ for r_ in b.readers if r_.eng != eng)
        for b in wb:
            if b.multi:
                if b.readers:
                    deps.extend(b.readers)
                    deps.extend(b.writers)
                    b.writers = [op]
                    b.readers = []
                else:
                    self._push(b.writers, op)
            else:
                deps.extend(b.writers)
                deps.extend(b.readers)
                b.writers = [op]
                b.readers = []
        for b in rb:
            self._push(b.readers, op)
        dd = []
        for d in deps:
            if d is op or d in dd:
                continue
            if (not d.is_dma) and (not is_dma) and d.eng == eng and (eng == "pe" or not SAME_SYNC):
                continue
            d.ms = True
            dd.append(d)
        op.deps = dd
        self.ops[eng].append(op)
        self.nops += 1
        if is_dma:
            self.dma_since.append(op)
        return op

    @staticmethod
    def _push(lst, op):
        if not op.is_dma:
            for i, o in enumerate(lst):
                if (not o.is_dma) and o.eng == op.eng:
                    lst[i] = op
                    return
        lst.append(op)

    def barrier(self, reset_alloc=True):
        lasts = []
        for e in self.ENGS:
            for o in reversed(self.ops[e]):
                if not o.is_dma and o.fn is not None:
                    lasts.append(o)
                    break
        deps = lasts + self.dma_since
        for d in deps:
            d.ms = True
        for e in self.ENGS:
            op = Op()
            op.eng = e
            op.fn = None
            op.is_dma = False
            op.ms = False
            op.sem = None
            op.val = 0
            op.prev = 0
            op.deps = [d for d in deps if d.is_dma or d.eng != e or (SAME_SYNC and e != "pe")]
            self.ops[e].append(op)
        self.dma_since = []
        for b in self.bufs:
            b.writers = []
            b.readers = []
        if reset_alloc:
            self.sb_ptr = self.sb_base

    def dma(self, q, out, in_, final=False):
        o, i = out.ap, in_.ap
        op = self._add(q, lambda e: e.dma_start(out=o, in_=i), [in_], [out], is_dma=True)
        if final:
            self.final.append(op)
        return op

    def mm(self, out, lhsT, rhs, start, stop, extra_reads=()):
        o, l, r = out.ap, _mmap(lhsT), _mmap(rhs)
        return self._add("pe", lambda e: e.matmul(o, l, r, start=start, stop=stop),
                         [lhsT, rhs] + list(extra_reads), [out])

    def mm32(self, out, lhsT, rhs, start, stop):
        o, l, r = out.ap, lhsT.ap, rhs.ap
        return self._add("pe", lambda e: e.matmul(o, l, r, start=start, stop=stop), [lhsT, rhs], [out])

    def transpose(self, out, in_, ident):
        o, i, d = out.ap, in_.ap, ident.ap
        return self._add("pe", lambda e: e.transpose(o, i, d), [in_, ident], [out])

    def act(self, out, in_, func, bias=None, scale=1.0, accum=None):
        o, i = out.ap, in_.ap
        reads = [in_]
        kw = {}
        if isinstance(bias, V):
            reads.append(bias)
            kw["bias"] = bias.ap
        elif bias is not None:
            kw["bias"] = float(bias)
        if isinstance(scale, V):
            reads.append(scale)
            kw["scale"] = scale.ap
        else:
            kw["scale"] = float(scale)
        writes = [out]
        if accum is not None:
            kw["accum_out"] = accum.ap
            writes.append(accum)
        return self._add("act", lambda e: e.activation(o, i, func, **kw), reads, writes)

    def tt(self, eng, out, a, b, op):
        o, x, y = out.ap, a.ap, b.ap
        return self._add(eng, lambda e: e.tensor_tensor(o, x, y, op), [a, b], [out])

    def ts(self, eng, out, a, s1, s2, op0, op1=None):
        o, x = out.ap, a.ap
        reads = [a]
        c1 = s1.ap if isinstance(s1, V) else float(s1)
        if isinstance(s1, V):
            reads.append(s1)
        c2 = None
        if s2 is not None:
            c2 = s2.ap if isinstance(s2, V) else float(s2)
            if isinstance(s2, V):
                reads.append(s2)
        if op1 is None:
            return self._add(eng, lambda e: e.tensor_scalar(o, x, c1, None, op0), reads, [out])
        return self._add(eng, lambda e: e.tensor_scalar(o, x, c1, c2, op0, op1), reads, [out])

    def stt(self, eng, out, a, s, b, op0, op1):
        o, x, y = out.ap, a.ap, b.ap
        reads = [a, b]
        c = s.ap if isinstance(s, V) else float(s)
        if isinstance(s, V):
            reads.append(s)
        return self._add(eng, lambda e: e.scalar_tensor_tensor(o, x, c, y, op0, op1), reads, [out])

    def copy(self, eng, out, in_):
        o, i = out.ap, in_.ap
        if eng == "act":
            return self._add("act", lambda e: e.activation(o, i, AF.Copy), [in_], [out])
        return self._add(eng, lambda e: e.tensor_copy(o, i), [in_], [out])

    def memset(self, eng, out, val):
        o = out.ap
        return self._add(eng, lambda e: e.memset(o, val), [], [out])

    def generic(self, eng, fn, reads, writes):
        return self._add(eng, fn, reads, writes)

    def finalize(self, es):
        nc = self.nc
        comp = ("act", "dve", "pool", "pe")
        sems = {e: es.enter_context(nc.semaphore(f"s_{e}")) for e in comp}
        rings = {q: [es.enter_context(nc.semaphore(f"r_{q}{i}")) for i in range(RING)]
                 for q in ("sp", "act", "pool")}
        for e in self.ENGS:
            cnt = 0
            n = 0
            for op in self.ops[e]:
                if op.fn is None:
                    continue
                if op.is_dma:
                    op.sem = rings[e][n % RING]
                    op.val = 16 * (n // RING + 1)
                    op.prev = 16 * (n // RING)
                    n += 1
                elif op.ms:
                    cnt += 1
                    op.sem = sems[e]
                    op.val = cnt
        block = es.enter_context(nc.Block())
        names = {"sp": "sync", "act": "scalar", "dve": "vector", "pool": "gpsimd", "pe": "tensor"}
        final = self.final

        def make(e):
            def body(engine):
                known = {}
                semobj = {}

                def need(d):
                    k = id(d.sem)
                    semobj[k] = d.sem
                    return k

                for op in self.ops[e]:
                    waits = {}
                    for d in op.deps:
                        k = need(d)
                        if waits.get(k, 0) < d.val:
                            waits[k] = d.val
                    if op.is_dma and op.prev > 0:
                        k = id(op.sem)
                        semobj[k] = op.sem
                        if waits.get(k, 0) < op.prev:
                            waits[k] = op.prev
                    for k, val in waits.items():
                        if known.get(k, 0) < val:
                            engine.wait_ge(semobj[k], val)
                            known[k] = val
                    if op.fn is None:
                        continue
                    ins = op.fn(engine)
                    if op.is_dma:
                        ins.then_inc(op.sem, 16)
                    elif op.ms:
                        ins.then_inc(op.sem, 1)
                if e == "sp":
                    for d in final:
                        k = need(d)
                        if known.get(k, 0) < d.val:
                            engine.wait_ge(d.sem, d.val)
                            known[k] = d.val
            return body

        for e in self.ENGS:
            getattr(block, names[e])(make(e))


class Model:
    def __init__(self, P, layers, dbg=None, nb=NB, first_in=None):
        self.P = P
        self.layers = layers
        self.dbg = dbg or {}
        self.nb = nb
        self.inputs = {}
        self.outputs = {}

    def ext_in(self, name, shape):
        self.inputs[name] = tuple(shape)
        return self.P.dram(name, shape, kind="ExternalInput", tracked=False)

    def ext_out(self, name, shape):
        self.outputs[name] = tuple(shape)
        return self.P.dram(name, shape, kind="ExternalOutput", tracked=True)

    def wtile(self):
        t = self.wt[self.wt_i % len(self.wt)]
        self.wt_i += 1
        return t

    def setup(self):
        P = self.P
        nb = self.nb
        self.hT0 = self.ext_in("hT0", [nb, D, T])
        if 3 in self.layers:
            self.hT = P.dram("hT", [nb, D, T])
        else:
            self.hT = self.ext_out("hT_out", [nb, D, T])
        self.oT = P.dram("oT", [nb, D, T])
        self.fTM = P.dram("fTM", [nb, T, D])
        self.rank_d = P.dram("rank_d", [nb, 16, T])
        self.rankT_d = P.dram("rankT_d", [nb, T, 16])
        self.yg_d = P.dram("yg_d", [nb, 16, 288, D])
        self.iota_row = self.ext_in("iota_row", [128, 288])
        self.iota_col = self.ext_in("iota_col", [128, 4])
        self.Gd = P.dram("Gd", [nb, 16, T])
        self.cvec = self.ext_in("cvec", [128, 16, 4])
        self.ada_w = self.ext_in("ada_w", [len(self.layers), D, 6 * D])
        self.ada_bT = self.ext_in("ada_bT", [len(self.layers), 128, 96])
        self.normgT = self.ext_in("normgT", [len(self.layers), 2, 128, 16])
        self.final_gT = self.ext_in("final_gT", [128, 16])
        self.router = self.ext_in("router", [len(self.layers), D, 16])
        self.w1 = self.ext_in("moe_w1", [len(self.layers), 16, D, 1024])
        self.w3 = self.ext_in("moe_w3", [len(self.layers), 16, D, 1024])
        self.w2 = self.ext_in("moe_w2", [len(self.layers), 16, 1024, D])
        self.identd = self.ext_in("ident", [128, 128])
        if 3 in self.layers:
            self.outT = self.ext_out("outT", [nb, D, NLAT])
        kinds = set(li % 4 for li in self.layers)
        if 0 in kinds or 1 in kinds:
            self.distf = self.ext_in("dist_f", [T, T])
            self.distb = self.ext_in("dist_b", [T, T])
        self.ones = P.tile("ones", [128], persistent=True)
        self.ident = P.tile("ident", [128], persistent=True)
        self.s_sb = P.tile("s_sb", [16, 4], persistent=True)
        self.mod = P.tile("mod", [96, 4], persistent=True)
        self.gsc = [P.tile(f"gsc{i}", [16, 4], persistent=True) for i in range(2)]
        self.ng = P.tile("ng", [2, 16], persistent=True)
        self.fg = P.tile("fg", [16], persistent=True)
        self.affT = [P.tile(f"affT{b}", [T], persistent=True, nparts=16) for b in range(nb)]
        self.eps_t = P.tile("eps", [1], persistent=True)
        P.memset("dve", self.ones, 1.0)
        P.memset("dve", self.eps_t, EPS)
        P.dma("sp", self.ident, self.identd)
        P.dma("sp", self.fg, self.final_gT)
        ctmp = P.tile("ctmp", [16, 4])
        P.dma("sp", ctmp, self.cvec)
        P.act(self.s_sb, ctmp, AF.Silu)
        P.barrier()

    def phase_mods(self, li):
        P = self.P
        self.wt = [P.tile(f"wt{i}", [16, 512]) for i in range(2)]
        self.wt_i = 0
        bT = P.tile("bT", [96])
        P.dma("sp", bT, self.ada_bT[li])
        P.dma("sp", self.ng, self.normgT[li].rr("w p c -> p w c"))
        for blk in range(24):
            w = self.wtile()
            P.dma("sp", w, self.ada_w[li][:, blk * 512:(blk + 1) * 512].rr("(kc p) n -> p kc n", p=128))
            for j in range(4):
                oc = blk * 4 + j
                ps = P.bank()
                for kc in range(16):
                    P.mm32(ps[:, 0:4], w[:, kc, j * 128:(j + 1) * 128], self.s_sb[:, kc, :], kc == 0, kc == 15)
                P.ts("dve", self.mod[:, oc, :], ps[:, 0:4], bT[:, oc:oc + 1], None, ALU.add)
        for w_i, sc_idx in ((0, 1), (1, 4)):
            tmp = P.tile(f"gtmp{w_i}", [16, 4])
            P.ts("dve", tmp, self.mod[:, sc_idx * 16:(sc_idx + 1) * 16, :], 1.0, None, ALU.add)
            P.tt("dve", self.gsc[w_i], tmp, self.ng[:, w_i, :].un(2).bc([128, 16, 4]), ALU.mult)
        P.barrier()

    def rsqrt(self, out, in_, scale, tmp):
        P = self.P
        P.act(tmp, in_, AF.Sqrt, bias=self.eps_t, scale=scale)
        o, i = out.ap, tmp.ap
        P.generic("dve", lambda e: e.reciprocal(o, i), [tmp], [out])

    def modv(self, which, kc, j):
        return self.mod[:, which * 16 + kc, j:j + 1]

    def norm_mod(self, h, a, n, w_i, j, rs, plain_g=None):
        P = self.P
        P.act(a[:, :, 0:n], h[:, :, 0:n], AF.Square)
        ps = P.bank()
        for kc in range(16):
            P.mm32(ps[:, 0:n], self.ones, a[:, kc, 0:n], kc == 0, kc == 15)
        rstd = rs[:, 0, 0:n]
        self.rsqrt(rstd, ps[:, 0:n], 1.0 / D, rs[:, 1, 0:n])
        if plain_g is not None:
            for kc in range(16):
                P.stt("dve", a[:, kc, 0:n], h[:, kc, 0:n], plain_g[:, kc:kc + 1], rstd, ALU.mult, ALU.mult)
            return
        sh_idx = 0 if w_i == 0 else 3
        for kc in range(16):
            P.stt("dve", a[:, kc, 0:n], h[:, kc, 0:n], self.gsc[w_i][:, kc, j:j + 1], rstd, ALU.mult, ALU.mult)
            P.act(a[:, kc, 0:n], a[:, kc, 0:n], AF.Identity, bias=self.modv(sh_idx, kc, j), scale=1.0)

    def linear_fm(self, a, n, W, ocs, cb, kcs=16):
        P = self.P
        nblk = (ocs + 3) // 4
        for blk in range(nblk):
            w = self.wtile()
            ncol = min(512, ocs * 128 - blk * 512)
            P.dma("sp", w[:, 0:kcs, 0:ncol], W[:, blk * 512:blk * 512 + ncol].rr("(kc p) n -> p kc n", p=128))
            for j in range(ncol // 128):
                oc = blk * 4 + j
                ps = P.bank()
                for kc in range(kcs):
                    P.mm(ps[:, 0:n], w[:, kc, j * 128:(j + 1) * 128], a[:, kc, 0:n], kc == 0, kc == kcs - 1)
                cb(oc, ps)

    def linear_tm(self, a, n, W, ncols, cb, kcs=16):
        P = self.P
        for blk in range((ncols + 511) // 512):
            w = self.wtile()
            ncol = min(512, ncols - blk * 512)
            P.dma("sp", w[:, 0:kcs, 0:ncol], W[:, blk * 512:blk * 512 + ncol].rr("(kc p) n -> p kc n", p=128))
            for ts in range(n // 128):
                ps = P.bank()
                for kc in range(kcs):
                    P.mm(ps[:, 0:ncol], a[:, kc, ts * 128:(ts + 1) * 128], w[:, kc, 0:ncol], kc == 0, kc == kcs - 1)
                cb(blk, ts, ps)

    def setup_ret(self):
        self.ret = dict(
            wq=self.ext_in("ret_wq", [D, D]), wk=self.ext_in("ret_wk", [D, D]), wv=self.ext_in("ret_wv", [D, D]),
            wg=self.ext_in("ret_wg", [D, D]), wo=self.ext_in("ret_wo", [D, D]),
            decay_b=self.ext_in("ret_decay_b", [128, 16]), gnw=self.ext_in("ret_gn_wT", [128, 16]),
            gnb=self.ext_in("ret_gn_bT", [128, 16]), cos=self.ext_in("rope_cosT", [128, NLAT]),
            sin=self.ext_in("rope_sinT", [128, NLAT]))

    def alloc_qkv(self, dq):
        P = self.P
        nb = self.nb
        if not hasattr(self, "qT"):
            self.qT = P.dram("qT", [nb, D, T])
            self.kT = P.dram("kT", [nb, D, T])
            self.vv = P.dram("vv", [nb, T, D])
            self.gT = P.dram("gT", [nb, D, T])

    def proj_common(self, li, per_tile):
        P = self.P
        self.wt = [P.tile(f"wt{i}", [16, 512]) for i in range(2)]
        self.wt_i = 0
        h = P.tile("h", [16, 512])
        a = P.tile("a", [16, 512])
        rs = P.tile("rs", [2, 512])
        src = self.hT0 if li == self.layers[0] else self.hT
        for b in range(self.nb):
            for ti, (t0, n) in enumerate(TILES):
                P.dma("sp", h[:, :, 0:n], src[b][:, t0:t0 + n].rr("(kc p) t -> p kc t", p=128))
                j = 2 if ti == 0 else b
                self.norm_mod(h, a, n, 0, j, rs)
                per_tile(b, ti, t0, n, a)

    def store_fm(self, dst, b, oc0, noc, t0, n, stage):
        self.P.dma("pool", dst[b][oc0 * 128:(oc0 + noc) * 128, t0:t0 + n].rr("(j p) t -> p j t", p=128),
                   stage[:, 0:noc, 0:n])

    def phase_proj_ret(self, li):
        P = self.P
        R = self.ret
        self.alloc_qkv(D)
        stg = [P.tile(f"stg{i}", [4, 512]) for i in range(2)]
        stv = [P.tile(f"stv{i}", [512]) for i in range(2)]
        cs = P.tile("cs", [2, 512])
        xs = P.tile("xs", [2, 512])
        tm = P.tile("tm", [4, 512])
        st_i = [0, 0]

        def per_tile(b, ti, t0, n, a):
            lat = ti > 0
            if lat:
                P.dma("sp", cs[:, 0, 0:n], R["cos"][:, t0 - LCTX:t0 - LCTX + n])
                P.dma("sp", cs[:, 1, 0:n], R["sin"][:, t0 - LCTX:t0 - LCTX + n])
            for W, dst, scale in ((R["wq"], self.qT, 1.0), (R["wk"], self.kT, 1.0 / 16.0)):
                def cb(oc, ps, dst=dst, scale=scale):
                    j = oc % 4
                    if j == 0:
                        st_i[0] += 1
                    stage = stg[st_i[0] % 2]
                    if not lat:
                        P.act(stage[:, j, 0:n], ps[:, 0:n], AF.Copy, scale=scale)
                    else:
                        half = oc % 2
                        P.act(xs[:, half, 0:n], ps[:, 0:n], AF.Copy, scale=scale)
                        if half == 1:
                            c_, s_ = cs[:, 0, 0:n], cs[:, 1, 0:n]
                            x1, x2 = xs[:, 0, 0:n], xs[:, 1, 0:n]
                            P.tt("dve", tm[:, 0, 0:n], x1, c_, ALU.mult)
                            P.tt("pool", tm[:, 1, 0:n], x2, s_, ALU.mult)
                            P.tt("dve", stage[:, j - 1, 0:n], tm[:, 0, 0:n], tm[:, 1, 0:n], ALU.subtract)
                            P.tt("dve", tm[:, 2, 0:n], x1, s_, ALU.mult)
                            P.tt("pool", tm[:, 3, 0:n], x2, c_, ALU.mult)
                            P.tt("dve", stage[:, j, 0:n], tm[:, 2, 0:n], tm[:, 3, 0:n], ALU.add)
                    if j == 3:
                        self.store_fm(dst, b, oc - 3, 4, t0, n, stage)
                self.linear_fm(a, n, W, 16, cb)

            def cbg(oc, ps):
                j = oc % 4
                if j == 0:
                    st_i[0] += 1
                stage = stg[st_i[0] % 2]
                P.act(stage[:, j, 0:n], ps[:, 0:n], AF.Silu)
                if j == 3:
                    self.store_fm(self.gT, b, oc - 3, 4, t0, n, stage)
            self.linear_fm(a, n, R["wg"], 16, cbg)

            def cbv(blk, ts, ps):
                st_i[1] += 1
                sv = stv[st_i[1] % 2]
                P.copy("act", sv, ps)
                P.dma("pool", self.vv[b][t0 + ts * 128:t0 + (ts + 1) * 128, blk * 512:(blk + 1) * 512], sv)
            self.linear_tm(a, n, R["wv"], D, cbv)

        self.proj_common(li, per_tile)
        P.barrier()

    def phase_attn_ret(self):
        P = self.P
        R = self.ret
        B = P.banks
        lg = P.tile("lg", [16])
        gnw = P.tile("gnw", [16])
        gnb = P.tile("gnb", [16])
        P.dma("sp", lg, R["decay_b"])
        P.dma("sp", gnw, R["gnw"])
        P.dma("sp", gnb, R["gnb"])
        P.act(lg, lg, AF.Sigmoid)
        P.act(lg, lg, AF.Ln)
        KT = P.tile("KT", [2, T])
        QT = P.tile("QT", [2, T])
        Vt = P.tile("Vt", [18, 256])
        e1 = [P.tile(f"e1_{i}", [512]) for i in range(2)]
        e2 = [P.tile(f"e2_{i}", [512]) for i in range(2)]
        pt = [P.tile(f"pt_{i}", [512]) for i in range(2)]
        df = [P.tile(f"df_{i}", [512]) for i in range(2)]
        db = [P.tile(f"db_{i}", [512]) for i in range(2)]
        osb = P.tile("osb", [2, 512])
        cen = P.tile("cen", [2, 512])
        sq = P.tile("sq", [2, 512])
        rs2 = P.tile("rs2", [2, 512])
        gt = P.tile("gt", [2, 512])
        yo = P.tile("yo", [2, 512])
        it = 0
        for b in range(self.nb):
            for h in range(8):
                P.dma("sp", KT, self.kT[b][h * 256:(h + 1) * 256, :].rr("(c p) t -> p c t", p=128))
                P.dma("sp", QT, self.qT[b][h * 256:(h + 1) * 256, :].rr("(c p) t -> p c t", p=128))
                P.dma("sp", Vt, self.vv[b][:, h * 256:(h + 1) * 256].rr("(st p) d -> p st d", p=128))
                for (t0, n) in TILES:
                    sts = [st for st in range(18) if not (st >= 2 and t0 < LCTX)]
                    O = [B[2], B[3]]
                    for i, st in enumerate(sts):
                        S = B[i % 2]
                        k = it % 2
                        it += 1
                        for c in range(2):
                            P.mm(S[:, 0:n], KT[:, c, st * 128:(st + 1) * 128], QT[:, c, t0:t0 + n], c == 0, c == 1)
                        P.dma("sp", df[k][:, 0:n], self.distf[st * 128:(st + 1) * 128, t0:t0 + n])
                        P.dma("sp", db[k][:, 0:n], self.distb[st * 128:(st + 1) * 128, t0:t0 + n])
                        P.act(e1[k][:, 0:n], df[k][:, 0:n], AF.Exp, scale=lg[:, h:h + 1])
                        P.act(e2[k][:, 0:n], db[k][:, 0:n], AF.Exp, scale=lg[:, 8 + h:9 + h])
                        P.tt("pool", e1[k][:, 0:n], e1[k][:, 0:n], e2[k][:, 0:n], ALU.add)
                        P.tt("dve", pt[k][:, 0:n], e1[k][:, 0:n], S[:, 0:n], ALU.mult)
                        for c in range(2):
                            P.mm(O[c][:, 0:n], Vt[:, st, c * 128:(c + 1) * 128], pt[k][:, 0:n], i == 0, i == len(sts) - 1)
                    self.head_norm_store(b, h * 2, 2, t0, n, O, osb, cen, sq, rs2, gt, yo, gnw, gnb, True, B[4], B[5])
        P.barrier()

    def head_norm_store(self, b, c0, nc_, t0, n, O, osb, cen, sq, rs2, gt, yo, gnw, gnb, center, bm, bv, src_sb=None):
        P = self.P
        dh = nc_ * 128
        if src_sb is None:
            for c in range(nc_):
                P.copy("act", osb[:, c, 0:n], O[c][:, 0:n])
        else:
            osb = src_sb
        if center:
            for c in range(nc_):
                P.mm32(bm[:, 0:n], self.ones, osb[:, c, 0:n], c == 0, c == nc_ - 1)
            for c in range(nc_):
                P.stt("dve", cen[:, c, 0:n], bm[:, 0:n], -1.0 / dh, osb[:, c, 0:n], ALU.mult, ALU.add)
        else:
            cen = osb
        P.act(sq[:, 0:nc_, 0:n], cen[:, 0:nc_, 0:n], AF.Square)
        for c in range(nc_):
            P.mm32(bv[:, 0:n], self.ones, sq[:, c, 0:n], c == 0, c == nc_ - 1)
        rstd = rs2[:, 0, 0:n]
        self.rsqrt(rstd, bv[:, 0:n], 1.0 / dh, rs2[:, 1, 0:n])
        P.dma("sp", gt[:, 0:nc_, 0:n], self.gT[b][c0 * 128:(c0 + nc_) * 128, t0:t0 + n].rr("(c p) t -> p c t", p=128))
        for c in range(nc_):
            P.stt("dve", yo[:, c, 0:n], cen[:, c, 0:n], gnw[:, c0 + c:c0 + c + 1], rstd, ALU.mult, ALU.mult)
            if gnb is not None:
                P.act(yo[:, c, 0:n], yo[:, c, 0:n], AF.Identity, bias=gnb[:, c0 + c:c0 + c + 1], scale=1.0)
            P.tt("pool", yo[:, c, 0:n], yo[:, c, 0:n], gt[:, c, 0:n], ALU.mult)
        P.dma("pool", self.oT[b][c0 * 128:(c0 + nc_) * 128, t0:t0 + n].rr("(c p) t -> p c t", p=128), yo[:, 0:nc_, 0:n])

    def setup_ml(self):
        self.ml = dict(
            wq=self.ext_in("ml_wq", [D, 1024]), wk=self.ext_in("ml_wk", [D, 1024]), wv=self.ext_in("ml_wv", [D, D]),
            wog=self.ext_in("ml_wog", [D, D]), wout=self.ext_in("ml_wout", [D, D]),
            wgi=self.ext_in("ml_wgi", [D, 16]), wgf=self.ext_in("ml_wgf", [D, 16]),
            bg=self.ext_in("ml_bg", [16, 2]), mlc=self.ext_in("ml_c", [16, 2]),
            selm=self.ext_in("ml_selm", [16, 16 * 128]), nw=self.ext_in("ml_norm_wT", [128, 16]))
        self.gates_d = self.P.dram("gates_d", [self.nb, 2, 16, T])

    def phase_proj_ml(self, li):
        P = self.P
        R = self.ml
        self.alloc_qkv(D)
        stg = [P.tile(f"stg{i}", [4, 512]) for i in range(2)]
        stv = [P.tile(f"stv{i}", [512]) for i in range(2)]
        wg = P.tile("wg", [2, 16, 16])
        bg = P.tile("bg", [2], nparts=16)
        grow = P.tile("grow", [2, 512], nparts=16)
        P.dma("sp", wg[:, 0], R["wgi"].rr("(kc p) e -> p kc e", p=128))
        P.dma("sp", wg[:, 1], R["wgf"].rr("(kc p) e -> p kc e", p=128))
        P.dma("sp", bg, R["bg"])
        st_i = [0, 0]

        def per_tile(b, ti, t0, n, a):
            for W, dst, scale in ((R["wq"], self.qT, 1.0), (R["wk"], self.kT, 128.0 ** -0.5)):
                def cb(oc, ps, dst=dst, scale=scale):
                    j = oc % 4
                    if j == 0:
                        st_i[0] += 1
                    stage = stg[st_i[0] % 2]
                    P.act(stage[:, j, 0:n], ps[:, 0:n], AF.Copy, scale=scale)
                    if j == 3:
                        self.store_fm(dst, b, oc - 3, 4, t0, n, stage)
                self.linear_fm(a, n, W, 8, cb)

            def cbg(oc, ps):
                j = oc % 4
                if j == 0:
                    st_i[0] += 1
                stage = stg[st_i[0] % 2]
                P.act(stage[:, j, 0:n], ps[:, 0:n], AF.Sigmoid)
                if j == 3:
                    self.store_fm(self.gT, b, oc - 3, 4, t0, n, stage)
            self.linear_fm(a, n, R["wog"], 16, cbg)

            def cbv(blk, ts, ps):
                st_i[1] += 1
                sv = stv[st_i[1] % 2]
                P.copy("act", sv, ps)
                P.dma("pool", self.vv[b][t0 + ts * 128:t0 + (ts + 1) * 128, blk * 512:(blk + 1) * 512], sv)
            self.linear_tm(a, n, R["wv"], D, cbv)
            for w_ in range(2):
                ps = P.bank()
                for kc in range(16):
                    P.mm32(ps[0:16, 0:n], wg[:, w_, kc, :], a[:, kc, 0:n], kc == 0, kc == 15)
                P.act(grow[:, w_, 0:n], ps[0:16, 0:n], AF.Identity, bias=bg[:, w_:w_ + 1], scale=1.0)
            P.dma("pool", self.gates_d[b][:, :, t0:t0 + n].rr("w r t -> r w t"), grow[:, :, 0:n])

        self.proj_common(li, per_tile)
        P.barrier()

    def phase_attn_ml(self):
        P = self.P
        R = self.ml
        B = P.banks
        df_h, db_h = dist_tables()
        dists = (self.distf, self.distb)

        def validity(dh, st, t0, n):
            blk = dh[st * 128:(st + 1) * 128, t0:t0 + n] < 1e8
            return 0 if not blk.any() else (2 if blk.all() else 1)

        nw = P.tile("nw", [16])
        P.dma("sp", nw, R["nw"])
        mlc = P.tile("mlc", [2], nparts=16)
        P.dma("sp", mlc, R["mlc"])
        selm = P.tile("selm", [16, 128], nparts=16)
        P.dma("sp", selm, R["selm"].rr("r (k m) -> r k m", m=128))
        Ir = P.tile("Ir", [T], nparts=16)
        Fr = P.tile("Fr", [T], nparts=16)
        pre = P.tile("pre", [T], nparts=16)
        Ph = P.tile("Ph", [T], nparts=16)
        onr = P.tile("onr", [T], nparts=16)
        gam = P.tile("gam", [2], nparts=16)
        icol = P.tile("icol", [18, 16])
        KT = P.tile("KT", [T])
        QT = P.tile("QT", [T])
        Vt = P.tile("Vt", [18, 256])
        phib = [P.tile(f"phib{i}", [512]) for i in range(2)]
        dtl = [P.tile(f"dtl{i}", [512]) for i in range(2)]
        wtl = [P.tile(f"wtl{i}", [512]) for i in range(2)]
        ptl = [P.tile(f"ptl{i}", [512]) for i in range(2)]
        dn = P.tile("dn", [2, 512])
        osb = P.tile("osb", [2, 512])
        hb_ = P.tile("hb_", [2, 512])
        cen = P.tile("cen", [2, 512])
        sq = P.tile("sq", [2, 512])
        rs2 = P.tile("rs2", [2, 512])
        gt = P.tile("gt", [2, 512])
        yo = P.tile("yo", [2, 512])
        P.memset("dve", onr, 1.0)
        alpha, beta = mlc[:, 0:1], mlc[:, 1:2]
        it = 0
        for b in range(self.nb):
            P.dma("sp", Ir, self.gates_d[b][0])
            P.dma("sp", Fr, self.gates_d[b][1])
            P.act(Fr, Fr, AF.Sigmoid)
            P.act(Fr, Fr, AF.Ln)
            for (c0, c1) in ((0, LCTX), (LCTX, T)):
                po, d0, d1 = pre.ap[:, c0:c1], onr.ap[:, c0:c1], Fr.ap[:, c0:c1]
                P.generic("dve", lambda e, po=po, d0=d0, d1=d1: e.tensor_tensor_scan(po, d0, d1, 0.0, ALU.mult, ALU.add),
                          [onr, Fr], [pre])
            P.ts("dve", gam[:, 0:1], pre[:, LCTX - 1:LCTX], beta, None, ALU.mult)
            P.stt("dve", gam[:, 1:2], pre[:, T - 1:T], beta, pre[:, LCTX - 1:LCTX], ALU.mult, ALU.add)
            P.ts("dve", Ph[:, 0:LCTX], pre[:, 0:LCTX], alpha, gam[:, 0:1], ALU.mult, ALU.add)
            P.ts("dve", Ph[:, LCTX:T], pre[:, LCTX:T], alpha, gam[:, 1:2], ALU.mult, ALU.add)
            P.stt("dve", Ph, Fr, beta, Ph, ALU.mult, ALU.add)
            P.tt("dve", Ir, Ir, Ph, ALU.subtract)
            for st in range(18):
                pT = B[st % 2]
                P.transpose(pT[:, 0:16], Ir[:, st * 128:(st + 1) * 128], self.ident[0:16, 0:16])
                P.copy("act", icol[:, st, :], pT[:, 0:16])
            for h in range(8):
                P.dma("sp", KT, self.kT[b][h * 128:(h + 1) * 128, :])
                P.dma("sp", QT, self.qT[b][h * 128:(h + 1) * 128, :])
                P.dma("sp", Vt, self.vv[b][:, h * 256:(h + 1) * 256].rr("(st p) d -> p st d", p=128))
                for (t0, n) in TILES:
                    for d in range(2):
                        P.mm32(B[d][:, 0:n], selm[:, d * 8 + h, :], Ph[:, t0:t0 + n], True, True)
                        P.copy("act", phib[d][:, 0:n], B[d][:, 0:n])
                    blocks = []
                    for st in range(18):
                        v = [validity(df_h, st, t0, n), validity(db_h, st, t0, n)]
                        if v[0] or v[1]:
                            blocks.append((st, v))
                    cnt = [sum(1 for _, v in blocks if v[d]) for d in range(2)]
                    seen = [0, 0]
                    O = [[B[2], B[3]], [B[5], B[6]]]
                    DEN = [B[4], B[7]]
                    for i, (st, v) in enumerate(blocks):
                        S = B[i % 2]
                        P.mm(S[:, 0:n], KT[:, st * 128:(st + 1) * 128], QT[:, t0:t0 + n], True, True)
                        for d in range(2):
                            if not v[d]:
                                continue
                            k = it % 2
                            it += 1
                            src = phib[d][:, 0:n]
                            if v[d] == 1:
                                P.dma("sp", dtl[k][:, 0:n], dists[d][st * 128:(st + 1) * 128, t0:t0 + n])
                                P.ts("dve", dtl[k][:, 0:n], dtl[k][:, 0:n], 1e8, -1e30, ALU.is_ge, ALU.mult)
                                P.tt("dve", dtl[k][:, 0:n], dtl[k][:, 0:n], phib[d][:, 0:n], ALU.add)
                                src = dtl[k][:, 0:n]
                            P.act(wtl[k][:, 0:n], src, AF.Exp, bias=icol[:, st, d * 8 + h:d * 8 + h + 1], scale=1.0)
                            P.tt("dve", ptl[k][:, 0:n], wtl[k][:, 0:n], S[:, 0:n], ALU.mult)
                            first, lastf = seen[d] == 0, seen[d] == cnt[d] - 1
                            seen[d] += 1
                            for c in range(2):
                                P.mm(O[d][c][:, 0:n], Vt[:, st, c * 128:(c + 1) * 128], ptl[k][:, 0:n], first, lastf)
                            P.mm(DEN[d][:, 0:n], self.ones, ptl[k][:, 0:n], first, lastf)
                    for d in range(2):
                        P.act(dn[:, d, 0:n], DEN[d][:, 0:n], AF.Abs)
                        P.ts("dve", dn[:, d, 0:n], dn[:, d, 0:n], 1.0, None, ALU.max)
                        oo, ii = dn.ap[:, d, 0:n], dn.ap[:, d, 0:n]
                        P.generic("dve", lambda e, oo=oo, ii=ii: e.reciprocal(oo, ii), [dn], [dn])
                    for c in range(2):
                        P.tt("dve", osb[:, c, 0:n], O[0][c][:, 0:n], dn[:, 0, 0:n], ALU.mult)
                        P.tt("dve", hb_[:, c, 0:n], O[1][c][:, 0:n], dn[:, 1, 0:n], ALU.mult)
                        P.tt("pool", osb[:, c, 0:n], osb[:, c, 0:n], hb_[:, c, 0:n], ALU.add)
                    self.head_norm_store(b, h * 2, 2, t0, n, None, osb, cen, sq, rs2, gt, yo, nw, None, True, B[0], B[1], src_sb=osb)
        P.barrier()

    def setup_na(self):
        self.na = dict(wqkv=self.ext_in("na_wqkv", [D, 3 * D]), wo=self.ext_in("na_wo", [D, D]),
                       tab=self.ext_in("na_tab", [8, 16, 512, 64]))

    def phase_proj_na(self, li):
        P = self.P
        R = self.na
        self.alloc_qkv(D)
        stg = [P.tile(f"stg{i}", [4, 512]) for i in range(2)]
        stv = [P.tile(f"stv{i}", [512]) for i in range(2)]
        st_i = [0, 0]

        def per_tile(b, ti, t0, n, a):
            for W, dst in ((R["wqkv"][:, 0:D], self.qT), (R["wqkv"][:, D:2 * D], self.kT)):
                def cb(oc, ps, dst=dst):
                    j = oc % 4
                    if j == 0:
                        st_i[0] += 1
                    stage = stg[st_i[0] % 2]
                    P.copy("act", stage[:, j, 0:n], ps[:, 0:n])
                    if j == 3:
                        self.store_fm(dst, b, oc - 3, 4, t0, n, stage)
                self.linear_fm(a, n, W, 16, cb)

            def cbv(blk, ts, ps):
                st_i[1] += 1
                sv = stv[st_i[1] % 2]
                P.copy("act", sv, ps)
                P.dma("pool", self.vv[b][t0 + ts * 128:t0 + (ts + 1) * 128, blk * 512:(blk + 1) * 512], sv)
            self.linear_tm(a, n, R["wqkv"][:, 2 * D:3 * D], D, cbv)

        self.proj_common(li, per_tile)
        P.barrier()

    def phase_attn_na(self):
        P = self.P
        R = self.na
        B = P.banks
        sc = 128.0 ** -0.5
        KT = P.tile("KT", [T])
        QT = P.tile("QT", [T])
        Vt = P.tile("Vt", [18, 128])
        Vt2 = P.tile("Vt2", [17, 128])
        tab = P.tile("tab", [8, 4, 64])
        pw = [P.tile(f"pw{i}", [6, 64]) for i in range(2)]
        tmp = [P.tile(f"tmp{i}", [4, 64]) for i in range(2)]
        psm = [P.tile(f"psm{i}", [64]) for i in range(2)]
        rec = [P.tile(f"rec{i}", [64]) for i in range(2)]
        stage = [P.tile(f"stage{i}", [512]) for i in range(2)]
        pc = P.tile("pc", [2, 256])
        recc = P.tile("recc", [256])
        oc_ = P.tile("oc_", [256])
        for b in range(self.nb):
            for h in range(16):
                P.dma("sp", KT, self.kT[b][h * 128:(h + 1) * 128, :])
                P.dma("sp", QT, self.qT[b][h * 128:(h + 1) * 128, :])
                P.dma("sp", Vt, self.vv[b][:, h * 128:(h + 1) * 128].rr("(st p) d -> p st d", p=128))
                P.dma("sp", Vt2, self.vv[b][64:64 + 17 * 128, h * 128:(h + 1) * 128].rr("(st p) d -> p st d", p=128))
                for t_ in range(8):
                    P.dma("sp", tab[:, t_], R["tab"][t_, h].rr("(j p) q -> p j q", p=128))
                S = B[0]
                Sv = V(S.ap.rearrange("p (j q) -> p j q", q=256), S.buf)
                for st in range(2):
                    P.mm(Sv[:, st, :], KT[:, st * 128:(st + 1) * 128], QT[:, 0:LCTX], True, True)
                P.act(pc, Sv, AF.Exp, scale=sc)
                O, DEN = B[1], B[2]
                for st in range(2):
                    P.mm(O[:, 0:LCTX], Vt[:, st, :], pc[:, st, :], st == 0, st == 1)
                for st in range(2):
                    P.mm(DEN[:, 0:LCTX], self.ones, pc[:, st, :], st == 0, st == 1)
                ro, ri = recc.ap, DEN.ap[:, 0:LCTX]
                P.generic("dve", lambda e, ro=ro, ri=ri: e.reciprocal(ro, ri), [DEN], [recc])
                P.tt("dve", oc_, O[:, 0:LCTX], recc, ALU.mult)
                P.dma("pool", self.oT[b][h * 128:(h + 1) * 128, 0:LCTX], oc_)
                import os
                for r in range(int(os.environ.get("NA_ROWS", "32"))):
                    k = r % 2
                    r0 = min(max(r - 4, 0), 24)
                    rt = r if r <= 3 else (4 if r <= 28 else r - 24)
                    q0 = LCTX + r * 64
                    S = B[3 + (r % 2)]
                    Sv = V(S.ap[:, 0:384].rearrange("p (j q) -> p j q", q=64), S.buf)
                    kbase = LCTX + r0 * 64
                    vts = [Vt[:, 0, :], Vt[:, 1, :]]
                    for j in range(4):
                        if r0 % 2 == 0:
                            vts.append(Vt[:, 2 + r0 // 2 + j, :])
                        else:
                            vts.append(Vt2[:, (192 + r0 * 64) // 128 + j, :])
                    for j in range(6):
                        ks = j * 128 if j < 2 else kbase + (j - 2) * 128
                        P.mm(Sv[:, j, :], KT[:, ks:ks + 128], QT[:, q0:q0 + 64], True, True)
                    P.act(pw[k][:, 0:2, :], Sv[:, 0:2, :], AF.Exp, scale=sc)
                    P.stt("dve", tmp[k], Sv[:, 2:6, :], sc, tab[:, rt], ALU.mult, ALU.add)
                    P.act(pw[k][:, 2:6, :], tmp[k], AF.Exp)
                    O, DEN = B[5 + (r % 2)], B[7 if r % 2 else 1]
                    for j in range(6):
                        P.mm(O[:, 0:64], vts[j], pw[k][:, j, :], j == 0, j == 5)
                    for j in range(6):
                        P.mm(DEN[:, 0:64], self.ones, pw[k][:, j, :], j == 0, j == 5)
                    ro, ri = rec[k].ap, DEN.ap[:, 0:64]
                    P.generic("dve", lambda e, ro=ro, ri=ri: e.reciprocal(ro, ri), [DEN], [rec[k]])
                    stg_ = stage[(r // 8) % 2]
                    P.tt("dve", stg_[:, (r % 8) * 64:(r % 8 + 1) * 64], O[:, 0:64], rec[k], ALU.mult)
                    if r % 8 == 7:
                        P.dma("pool", self.oT[b][h * 128:(h + 1) * 128, LCTX + (r - 7) * 64:LCTX + (r + 1) * 64], stg_)
        P.barrier()

    def setup_hg(self):
        nb = self.nb
        self.hg = dict(wq=self.ext_in("hg_wq", [D, D]), wi=self.ext_in("hg_wi", [D, D]),
                       wf=self.ext_in("hg_wf", [2, D, D]), wg=self.ext_in("hg_wg", [D, D]),
                       wo=self.ext_in("hg_wo", [D, D]), nw=self.ext_in("hg_norm_wT", [128, 16]),
                       lb=self.ext_in("hg_lb", [4, D]), tri=self.ext_in("hg_tri", [4, 128, 128]))
        self.kk_d = self.P.dram("kk_d", [nb, 2, T, D])
        self.gl_d = self.P.dram("gl_d", [nb, 2, T, D])

    def phase_proj_hg(self, li):
        P = self.P
        R = self.hg
        self.alloc_qkv(D)
        stg = [P.tile(f"stg{i}", [4, 512]) for i in range(2)]
        stv = [P.tile(f"stv{i}", [512]) for i in range(6)]
        oml = P.tile("oml", [D])
        st_i = [0, 0]
        wt0 = P.tile("wt0", [16, 512])
        wt1 = P.tile("wt1", [16, 512])
        Ev = V(wt0.ap.rearrange("p a b -> p (a b)").rearrange("p (l d) -> p l d", l=4), wt0.buf)
        for l_ in range(4):
            P.dma("sp", Ev[:, l_, :], V(R["lb"].ap[l_:l_ + 1, :].partition_broadcast(128).rearrange("p a n -> p (a n)"), None))
        P.act(Ev, Ev, AF.Exp)
        P.tt("dve", oml, Ev[:, 0, :], Ev[:, 1, :], ALU.add)
        P.tt("dve", oml, oml, Ev[:, 2, :], ALU.add)
        P.tt("dve", oml, oml, Ev[:, 3, :], ALU.add)
        oo = oml.ap
        P.generic("dve", lambda e: e.reciprocal(oo, oo), [oml], [oml])
        P.tt("dve", oml, oml, Ev[:, 0, :], ALU.mult)

        def per_tile(b, ti, t0, n, a):
            for W, dst, fn, scale in ((R["wq"], self.qT, AF.Silu, 128.0 ** -0.5), (R["wg"], self.gT, AF.Silu, 1.0)):
                def cb(oc, ps, dst=dst, fn=fn, scale=scale):
                    j = oc % 4
                    if j == 0:
                        st_i[0] += 1
                    stage = stg[st_i[0] % 2]
                    P.act(stage[:, j, 0:n], ps[:, 0:n], fn)
                    if scale != 1.0:
                        P.ts("pool", stage[:, j, 0:n], stage[:, j, 0:n], scale, None, ALU.mult)
                    if j == 3:
                        self.store_fm(dst, b, oc - 3, 4, t0, n, stage)
                self.linear_fm(a, n, W, 16, cb)

            def cbv(blk, ts, ps):
                st_i[1] += 1
                sv = stv[st_i[1] % 6]
                P.copy("act", sv, ps)
                P.dma("pool", self.vv[b][t0 + ts * 128:t0 + (ts + 1) * 128, blk * 512:(blk + 1) * 512], sv)
            self.linear_tm(a, n, R["wi"], D, cbv)
            for d in range(2):
                def cbz(blk, ts, ps, d=d):
                    st_i[1] += 1
                    kt = stv[st_i[1] % 6]
                    st_i[1] += 1
                    gl = stv[st_i[1] % 6]
                    P.act(kt, ps, AF.Sigmoid, scale=-1.0)
                    P.tt("dve", kt, kt, oml[:, blk * 512:(blk + 1) * 512], ALU.mult)
                    P.dma("pool", self.kk_d[b, d][t0 + ts * 128:t0 + (ts + 1) * 128, blk * 512:(blk + 1) * 512], kt)
                    P.act(gl, kt, AF.Ln, bias=self.ones[:, 0:1], scale=-1.0)
                    P.dma("pool", self.gl_d[b, d][t0 + ts * 128:t0 + (ts + 1) * 128, blk * 512:(blk + 1) * 512], gl)
                self.linear_tm(a, n, R["wf"][d], D, cbz)

        self.wt = [wt0, wt1]
        self.wt_i = 0
        h = P.tile("h", [16, 512])
        a = P.tile("a", [16, 512])
        rs = P.tile("rs", [2, 512])
        src = self.hT0 if li == self.layers[0] else self.hT
        for b in range(self.nb):
            for ti, (t0, n) in enumerate(TILES):
                P.dma("sp", h[:, :, 0:n], src[b][:, t0:t0 + n].rr("(kc p) t -> p kc t", p=128))
                j = 2 if ti == 0 else b
                self.norm_mod(h, a, n, 0, j, rs)
                per_tile(b, ti, t0, n, a)
        P.barrier()

    def phase_attn_hg(self):
        P = self.P
        R = self.hg
        B = P.banks
        nw = P.tile("nw", [16])
        P.dma("sp", nw, R["nw"])
        tri = P.tile("tri", [4, 128])
        for i in range(4):
            P.dma("sp", tri[:, i, :], R["tri"][i])
        U, Ls, L, Us = tri[:, 0, :], tri[:, 1, :], tri[:, 2, :], tri[:, 3, :]
        QT = P.tile("QT", [T])
        Vt = P.tile("Vt", [18, 128])
        Kt = [P.tile(f"Kt{d}", [18, 128]) for d in range(2)]
        Gt = [P.tile(f"Gt{d}", [18, 128]) for d in range(2)]
        Oa = [P.tile(f"Oa{d}", [T]) for d in range(2)]
        Sst = [P.tile(f"S{d}", [128]) for d in range(2)]
        names = ("ebT", "enb", "eE", "dec", "qin", "kin", "kinT", "kend", "Am")
        tl = [{nm: P.tile(f"{nm}{d}", [128] if nm != "dec" else [1]) for nm in names} for d in range(2)]
        sq = P.tile("sq", [1, 512])
        rs2 = P.tile("rs2", [2, 512])
        gt = P.tile("gt", [1, 512])
        yo = P.tile("yo", [1, 512])
        orders = ([0, 1] + list(range(2, 18)), [1, 0] + list(range(17, 1, -1)))
        for b in range(self.nb):
            for h in range(16):
                P.dma("sp", QT, self.qT[b][h * 128:(h + 1) * 128, :])
                P.dma("sp", Vt, self.vv[b][:, h * 128:(h + 1) * 128].rr("(st p) d -> p st d", p=128))
                for d in range(2):
                    P.dma("sp", Kt[d], self.kk_d[b, d][:, h * 128:(h + 1) * 128].rr("(st p) d -> p st d", p=128))
                    P.dma("sp", Gt[d], self.gl_d[b, d][:, h * 128:(h + 1) * 128].rr("(st p) d -> p st d", p=128))
                for step in range(18):
                    for d in range(2):
                        c = orders[d][step]
                        tt_ = tl[d]
                        X0, X1, X2 = B[3 * d], B[3 * d + 1], B[3 * d + 2]
                        Mi, Me, Mk = (U, Ls, U) if d == 0 else (L, Us, L)
                        g_c, k_c, v_c = Gt[d][:, c, :], Kt[d][:, c, :], Vt[:, c, :]
                        cs = slice(c * 128, (c + 1) * 128)
                        bps, bTps, Eps, kTps = X0[:, 0:128], X0[:, 128:256], X0[:, 256:384], X0[:, 384:512]
                        P.mm32(bps, Mi, g_c, True, True)
                        P.mm32(bTps, g_c, Mi, True, True)
                        P.mm32(Eps, Me, g_c, True, True)
                        P.act(tt_["enb"], bps, AF.Exp, scale=-1.0)
                        P.act(tt_["ebT"], bTps, AF.Exp)
                        P.act(tt_["eE"], Eps, AF.Exp)
                        lastcol = bTps[:, 127:128] if d == 0 else bTps[:, 0:1]
                        P.act(tt_["dec"], lastcol, AF.Exp)
                        P.tt("dve", tt_["kin"], k_c, tt_["enb"], ALU.mult)
                        P.tt("pool", tt_["qin"], QT[:, cs], tt_["ebT"], ALU.mult)
                        P.tt("pool", tt_["kend"], k_c, tt_["eE"], ALU.mult)
                        P.transpose(kTps, tt_["kin"], self.ident)
                        P.copy("act", tt_["kinT"], kTps)
                        Aps, Sps = X1[:, 0:128], X1[:, 128:256]
                        P.mm32(Aps, tt_["kinT"], tt_["qin"], True, True)
                        P.tt("dve", tt_["Am"], Aps, Mk, ALU.mult)
                        ops_ = X2[:, 0:128]
                        P.mm32(ops_, v_c, tt_["Am"], True, step == 0)
                        if step > 0:
                            P.mm32(ops_, Sst[d], tt_["qin"], False, True)
                        P.copy("act", Oa[d][:, cs], ops_)
                        P.mm32(Sps, tt_["kend"], v_c, True, True)
                        if step == 0:
                            P.copy("dve", Sst[d], Sps)
                        else:
                            P.stt("dve", Sst[d], Sst[d], tt_["dec"][:, 0:1], Sps, ALU.mult, ALU.add)
                P.tt("dve", Oa[0], Oa[0], Oa[1], ALU.add)
                for (t0, n) in TILES:
                    osb = V(Oa[0].ap[:, t0:t0 + n].unsqueeze(1), Oa[0].buf)
                    self.head_norm_store(b, h, 1, t0, n, None, osb, None, sq, rs2, gt, yo, nw, None, False, B[6], B[7], src_sb=osb)
        P.barrier()

    def phase_out(self, idx, li, Wo, last):
        P = self.P
        self.wt = [P.tile(f"wt{i}", [16, 512]) for i in range(2)]
        self.wt_i = 0
        o = P.tile("o", [16, 512])
        h = P.tile("h", [16, 512])
        f = P.tile("f", [16, 512])
        rs = P.tile("rs", [2, 512])
        rw = P.tile("rw", [16, 16])
        lgt = P.tile("lgt", [16])
        ssum = P.tile("ssum", [1])
        ftm = [P.tile(f"ftm{i}", [512]) for i in range(2)]
        P.dma("sp", rw, self.router[idx].rr("(kc p) e -> p kc e", p=128))
        src = self.hT0 if li == self.layers[0] else self.hT
        for b in range(self.nb):
            for ti, (t0, n) in enumerate(TILES):
                if last and ti == 0:
                    continue
                j = 2 if ti == 0 else b
                P.dma("sp", o[:, :, 0:n], self.oT[b][:, t0:t0 + n].rr("(kc p) t -> p kc t", p=128))
                P.dma("sp", h[:, :, 0:n], src[b][:, t0:t0 + n].rr("(kc p) t -> p kc t", p=128))

                def cb(oc, ps):
                    P.stt("dve", h[:, oc, 0:n], ps[:, 0:n], self.modv(2, oc, j), h[:, oc, 0:n], ALU.mult, ALU.add)
                self.linear_fm(o, n, Wo, 16, cb)
                P.dma("pool", self.hT[b][:, t0:t0 + n].rr("(kc p) t -> p kc t", p=128), h[:, :, 0:n])
                self.norm_mod(h, f, n, 1, j, rs)
                for ts in range(n // 128):
                    for g4 in range(4):
                        pt_ = P.bank()
                        for jq in range(4):
                            P.transpose(pt_[:, jq * 128:(jq + 1) * 128], f[:, g4 * 4 + jq, ts * 128:(ts + 1) * 128], self.ident)
                        sg_ = ftm[(ts * 4 + g4) % 2]
                        P.copy("act", sg_, pt_)
                        P.dma("pool", self.fTM[b][t0 + ts * 128:t0 + (ts + 1) * 128, g4 * 512:(g4 + 1) * 512], sg_)
                for ts in range(n // 128):
                    ps = P.bank()
                    for kc in range(16):
                        P.mm32(ps[:, 0:16], f[:, kc, ts * 128:(ts + 1) * 128], rw[:, kc, :], kc == 0, kc == 15)
                    P.act(lgt, ps[:, 0:16], AF.Exp, accum=ssum)
                    oo, ii = ssum.ap, ssum.ap
                    P.generic("dve", lambda e, oo=oo, ii=ii: e.reciprocal(oo, ii), [ssum], [ssum])
                    P.ts("dve", lgt, lgt, ssum[:, 0:1], None, ALU.mult)
                    pT = P.bank()
                    P.transpose(pT[0:16, 0:128], lgt, self.ident)
                    P.copy("act", self.affT[b][:, t0 + ts * 128:t0 + (ts + 1) * 128], pT[0:16, 0:128])
        P.barrier()

    def phase_topk(self, last):
        P = self.P
        B = P.banks
        wk = P.tile("wk", [NLAT], nparts=16)
        m8 = P.tile("m8", [8], nparts=16)
        G = P.tile("G", [T], nparts=16)
        msk = P.tile("msk", [T], nparts=16)
        cum = P.tile("cum", [T], nparts=16)
        rk = P.tile("rk", [T], nparts=16)
        onr = P.tile("onr", [T], nparts=16)
        rkT = P.tile("rkT", [18, 16])
        P.memset("dve", onr, 1.0)
        for b in range(self.nb):
            for (c0, ncol, rounds, off) in ((LCTX, NLAT, 32, 0.0), (0, LCTX, 4, 256.0)):
                if last and c0 == 0:
                    P.memset("dve", G[:, 0:LCTX], 0.0)
                    P.memset("dve", rk[:, 0:LCTX], -1.0)
                    continue
                aff = self.affT[b][:, c0:c0 + ncol]
                w = wk[:, 0:ncol]
                P.copy("dve", w, aff)
                for r in range(rounds):
                    mo, wi = m8.ap, w.ap
                    P.generic("dve", lambda e, mo=mo, wi=wi: e.max(mo, wi), [w], [m8])
                    if r < rounds - 1:
                        P.generic("dve", lambda e, mo=mo, wi=wi: e.match_replace(wi, mo, wi, -1.0), [w, m8], [w])
                P.stt("dve", G[:, c0:c0 + ncol], aff, m8[:, 7:8], aff, ALU.is_ge, ALU.mult)
                P.ts("dve", msk[:, c0:c0 + ncol], aff, m8[:, 7:8], None, ALU.is_ge)
                po, d0, d1 = cum.ap[:, c0:c0 + ncol], onr.ap[:, c0:c0 + ncol], msk.ap[:, c0:c0 + ncol]
                P.generic("dve", lambda e, po=po, d0=d0, d1=d1: e.tensor_tensor_scan(po, d0, d1, 0.0, ALU.mult, ALU.add),
                          [onr, msk], [cum])
                P.stt("dve", rk[:, c0:c0 + ncol], cum[:, c0:c0 + ncol], off, msk[:, c0:c0 + ncol], ALU.add, ALU.mult)
                P.ts("dve", rk[:, c0:c0 + ncol], rk[:, c0:c0 + ncol], -1.0, None, ALU.add)
            P.dma("pool", self.Gd[b], G)
            P.dma("pool", self.rank_d[b], rk)
            for st in range(18):
                pT = B[st % 2]
                P.transpose(pT[:, 0:16], rk[:, st * 128:(st + 1) * 128], self.ident[0:16, 0:16])
                P.copy("act", rkT[:, st, :], pT[:, 0:16])
            P.dma("pool", self.rankT_d[b].rr("(st p) e -> p st e", p=128), rkT)
        P.barrier()

    def phase_moe_gather(self, idx, last):
        P = self.P
        self.wt = [P.tile(f"wt{i}", [16, 512]) for i in range(2)]
        self.wt_i = 0
        iota = P.tile("iota", [288])
        P.dma("sp", iota, self.iota_row)
        rkT = P.tile("rkT", [18, 16])
        selE = P.tile("selE", [18, 288])
        ft = [P.tile(f"ft{i}", [512]) for i in range(2)]
        xg = P.tile("xg", [16, 288])
        hm = P.tile("hm", [8, 288])
        su = [P.tile(f"su{i}", [288]) for i in range(2)]
        stg = [P.tile(f"stg{i}", [512]) for i in range(2)]
        k = 0
        NS = 288
        slot_tiles = [(0, 128), (128, 128)] + ([] if last else [(256, 32)])
        for b in range(self.nb):
            P.dma("sp", rkT, self.rankT_d[b].rr("(st p) e -> p st e", p=128))
            for e in range(16):
                st_list = list(range(2, 18)) if last else list(range(18))
                for st in st_list:
                    P.ts("dve" if st % 2 == 0 else "pool", selE[:, st, :], iota, rkT[:, st, e:e + 1], None, ALU.is_equal)
                for g4 in range(4):
                    bk = [P.bank() for _ in range(4)]
                    for st in st_list:
                        f_ = ft[k % 2]
                        k += 1
                        P.dma("sp", f_, self.fTM[b][st * 128:(st + 1) * 128, g4 * 512:(g4 + 1) * 512])
                        for jq in range(4):
                            P.mm(bk[jq][:, 0:NS], f_[:, jq * 128:(jq + 1) * 128], selE[:, st, :], st == st_list[0], st == 17)
                    for jq in range(4):
                        P.copy("act" if jq % 2 == 0 else "dve", xg[:, g4 * 4 + jq, :], bk[jq][:, 0:NS])
                for half in range(2):
                    w1t = self.wtile()
                    P.dma("sp", w1t, self.w1[idx, e][:, half * 512:(half + 1) * 512].rr("(kc p) n -> p kc n", p=128))
                    w3t = self.wtile()
                    P.dma("sp", w3t, self.w3[idx, e][:, half * 512:(half + 1) * 512].rr("(kc p) n -> p kc n", p=128))
                    for jj in range(4):
                        fc = half * 4 + jj
                        psu = P.bank()
                        for kc in range(16):
                            P.mm(psu[:, 0:NS], w1t[:, kc, jj * 128:(jj + 1) * 128], xg[:, kc, :], kc == 0, kc == 15)
                        psg = P.bank()
                        for kc in range(16):
                            P.mm(psg[:, 0:NS], w3t[:, kc, jj * 128:(jj + 1) * 128], xg[:, kc, :], kc == 0, kc == 15)
                        s_ = su[k % 2]
                        k += 1
                        P.act(s_, psu[:, 0:NS], AF.Silu)
                        P.tt("dve", hm[:, fc, :], s_, psg[:, 0:NS], ALU.mult)
                for blk in range(4):
                    w2t = self.wtile()
                    P.dma("sp", w2t[:, 0:8, :], self.w2[idx, e][:, blk * 512:(blk + 1) * 512].rr("(kc p) n -> p kc n", p=128))
                    for (s0, ns) in slot_tiles:
                        psy = P.bank()
                        for fc in range(8):
                            P.mm(psy[0:ns, :], hm[:, fc, s0:s0 + ns], w2t[:, fc, :], fc == 0, fc == 7)
                        sg_ = stg[k % 2]
                        k += 1
                        P.copy("act", sg_[0:ns, :], psy[0:ns, :])
                        P.dma("pool", self.yg_d[b, e][s0:s0 + ns, blk * 512:(blk + 1) * 512], sg_[0:ns, :])
        P.barrier()

    def phase_moe_scatter(self, idx, last):
        P = self.P
        ic = P.tile("ic", [4])
        P.dma("sp", ic, self.iota_col)
        SG = P.tile("SG", [16, 2, 512])
        rb = [P.tile(f"rb{i}", [512]) for i in range(2)]
        gb = [P.tile(f"gb{i}", [512]) for i in range(2)]
        ygc = [P.tile(f"ygc{i}", [16, 2, 128]) for i in range(2)]
        hf = P.tile("hf", [16, 512])
        ot = V(SG.ap.rearrange("p e s n -> p (e s n)")[:, 0:8192].rearrange("p (a b) -> p a b", b=512), SG.buf)
        rs = P.tile("rs", [2, 512])
        for b in range(self.nb):
            for ti, (t0, n) in enumerate(TILES):
                if last and ti == 0:
                    continue
                j = 2 if ti == 0 else b
                sts = [(2, 256, 32)] if ti == 0 else [(0, 0, 128), (1, 128, 128)]
                for e in range(16):
                    r_, g_ = rb[e % 2], gb[e % 2]
                    P.dma("sp", r_[:, 0:n], V(self.rank_d.ap[b, e:e + 1, t0:t0 + n].partition_broadcast(128).rearrange("p a n -> p (a n)"), self.rank_d.buf))
                    P.dma("sp", g_[:, 0:n], V(self.Gd.ap[b, e:e + 1, t0:t0 + n].partition_broadcast(128).rearrange("p a n -> p (a n)"), self.Gd.buf))
                    for si, (s_idx, s0, ns) in enumerate(sts):
                        P.stt("dve", SG[0:ns, e, si, 0:n], r_[0:ns, 0:n], ic[0:ns, s_idx:s_idx + 1], g_[0:ns, 0:n], ALU.is_equal, ALU.mult)
                P.dma("sp", hf[:, :, 0:n], self.hT[b][:, t0:t0 + n].rr("(kc p) t -> p kc t", p=128))
                for oc in range(16):
                    yc = ygc[oc % 2]
                    if ti == 0:
                        P.dma("sp", yc[0:32, :, 0, :], self.yg_d[b][:, 256:288, oc * 128:(oc + 1) * 128].rr("e p f -> p e f"))
                    else:
                        for si in range(2):
                            P.dma("sp", yc[:, :, si, :], self.yg_d[b][:, si * 128:(si + 1) * 128, oc * 128:(oc + 1) * 128].rr("e p f -> p e f"))
                    ps = P.bank()
                    cnt = 16 * len(sts)
                    i_ = 0
                    for e in range(16):
                        for si, (s_idx, s0, ns) in enumerate(sts):
                            P.mm(ps[:, 0:n], yc[0:ns, e, si, :], SG[0:ns, e, si, 0:n], i_ == 0, i_ == cnt - 1)
                            i_ += 1
                    P.stt("dve", hf[:, oc, 0:n], ps[:, 0:n], self.modv(5, oc, j), hf[:, oc, 0:n], ALU.mult, ALU.add)
                if not last:
                    P.dma("pool", self.hT[b][:, t0:t0 + n].rr("(kc p) t -> p kc t", p=128), hf[:, :, 0:n])
                else:
                    self.norm_mod(hf, ot, n, 0, 0, rs, plain_g=self.fg)
                    P.dma("pool", self.outT[b][:, t0 - LCTX:t0 - LCTX + n].rr("(kc p) t -> p kc t", p=128), ot[:, :, 0:n], final=True)
        P.barrier()

    def phase_moe(self, idx, last):
        P = self.P
        self.wt = [P.tile(f"wt{i}", [16, 512]) for i in range(2)]
        self.wt_i = 0
        f = P.tile("f", [16, 512])
        acc = P.tile("acc", [16, 512])
        hm = P.tile("hm", [8, 512])
        gbt = [P.tile(f"gbt{i}", [512]) for i in range(2)]
        su = [P.tile(f"su{i}", [512]) for i in range(2)]
        rs = P.tile("rs", [2, 512])
        k = 0
        for b in range(self.nb):
            for ti, (t0, n) in enumerate(TILES):
                if last and ti == 0:
                    continue
                j = 2 if ti == 0 else b
                P.dma("sp", f[:, :, 0:n], self.fT[b][:, t0:t0 + n].rr("(kc p) t -> p kc t", p=128))
                for e in range(16):
                    gb = gbt[e % 2]
                    P.dma("sp", gb[:, 0:n], V(self.Gd.ap[b, e:e + 1, t0:t0 + n].partition_broadcast(128).rearrange("p a n -> p (a n)"), self.Gd.buf))
                    for half in range(2):
                        w1t = self.wtile()
                        P.dma("sp", w1t, self.w1[idx, e][:, half * 512:(half + 1) * 512].rr("(kc p) n -> p kc n", p=128))
                        w3t = self.wtile()
                        P.dma("sp", w3t, self.w3[idx, e][:, half * 512:(half + 1) * 512].rr("(kc p) n -> p kc n", p=128))
                        for jj in range(4):
                            fc = half * 4 + jj
                            psu = P.bank()
                            for kc in range(16):
                                P.mm(psu[:, 0:n], w1t[:, kc, jj * 128:(jj + 1) * 128], f[:, kc, 0:n], kc == 0, kc == 15)
                            psg = P.bank()
                            for kc in range(16):
                                P.mm(psg[:, 0:n], w3t[:, kc, jj * 128:(jj + 1) * 128], f[:, kc, 0:n], kc == 0, kc == 15)
                            s_ = su[k % 2]
                            k += 1
                            P.act(s_[:, 0:n], psu[:, 0:n], AF.Silu)
                            P.tt("dve", hm[:, fc, 0:n], s_[:, 0:n], psg[:, 0:n], ALU.mult)
                            P.tt("pool", hm[:, fc, 0:n], hm[:, fc, 0:n], gb[:, 0:n], ALU.mult)
                    for blk in range(4):
                        w2t = self.wtile()
                        P.dma("sp", w2t[:, 0:8, :], self.w2[idx, e][:, blk * 512:(blk + 1) * 512].rr("(kc p) n -> p kc n", p=128))
                        for jj in range(4):
                            oc = blk * 4 + jj
                            psy = P.bank()
                            for fc in range(8):
                                P.mm(psy[:, 0:n], w2t[:, fc, jj * 128:(jj + 1) * 128], hm[:, fc, 0:n], fc == 0, fc == 7)
                            if e == 0:
                                P.copy("act", acc[:, oc, 0:n], psy[:, 0:n])
                            else:
                                P.tt("dve", acc[:, oc, 0:n], acc[:, oc, 0:n], psy[:, 0:n], ALU.add)
                P.dma("sp", f[:, :, 0:n], self.hT[b][:, t0:t0 + n].rr("(kc p) t -> p kc t", p=128))
                for oc in range(16):
                    P.stt("dve", f[:, oc, 0:n], acc[:, oc, 0:n], self.modv(5, oc, j), f[:, oc, 0:n], ALU.mult, ALU.add)
                if not last:
                    P.dma("pool", self.hT[b][:, t0:t0 + n].rr("(kc p) t -> p kc t", p=128), f[:, :, 0:n])
                else:
                    self.norm_mod(f, acc, n, 0, 0, rs, plain_g=self.fg)
                    P.dma("pool", self.outT[b][:, t0 - LCTX:t0 - LCTX + n].rr("(kc p) t -> p kc t", p=128), acc[:, :, 0:n], final=True)
        P.barrier()

    def emit_all(self):
        P = self.P
        self.setup_inputs_all()
        self.setup()
        nl = len(self.layers)
        for idx, li in enumerate(self.layers):
            kind = li % 4
            last = li == 3
            self.phase_mods(idx)
            if self.stop == "mods":
                self.dump("mod", self.mod_dump())
                return
            if kind == 0:
                self.phase_proj_ret(li)
                if self.stop == "proj":
                    for nm in ("qT", "kT", "gT", "vv"):
                        self.dump(nm, getattr(self, nm))
                    return
                self.phase_attn_ret()
                Wo = self.ret["wo"]
            elif kind == 1:
                self.phase_proj_ml(li)
                if self.stop == "proj":
                    for nm in ("qT", "kT", "gT", "vv", "gates_d"):
                        self.dump(nm, getattr(self, nm))
                    return
                self.phase_attn_ml()
                Wo = self.ml["wout"]
            elif kind == 3:
                self.phase_proj_hg(li)
                if self.stop == "proj":
                    for nm in ("qT", "gT", "vv", "kk_d", "gl_d"):
                        self.dump(nm, getattr(self, nm))
                    return
                self.phase_attn_hg()
                Wo = self.hg["wo"]
            elif kind == 2:
                self.phase_proj_na(li)
                if self.stop == "proj":
                    for nm in ("qT", "kT", "vv"):
                        self.dump(nm, getattr(self, nm))
                    return
                self.phase_attn_na()
                Wo = self.na["wo"]
            if self.stop == "attn":
                self.dump("oT", self.oT)
                return
            self.phase_out(idx, li, Wo, last)
            if self.stop == "out":
                self.dump("hT", self.hT)
                self.dump("fT", self.fT)
                return
            self.phase_topk(last)
            if self.stop == "topk":
                self.dump("Gd", self.Gd)
                return
            if os.environ.get("K_DENSE", "0") == "1":
                self.phase_moe(idx, last)
            else:
                self.phase_moe_gather(idx, last)
                self.phase_moe_scatter(idx, last)
            if self.stop == f"layer{li}":
                self.dump("hT", self.hT)
                return

    def mod_dump(self):
        d = self.P.dram("mod_d", [128, 96 * 4])
        self.P.dma("sp", d, self.mod.rr("p a b -> p (a b)"))
        return d

    def setup_inputs_all(self):
        kinds = set(li % 4 for li in self.layers)
        if 0 in kinds:
            self.setup_ret()
        if 1 in kinds:
            self.setup_ml()
        if 2 in kinds:
            self.setup_na()
        if 3 in kinds:
            self.setup_hg()

    def dump(self, name, view):
        o = self.ext_out("dbg_" + name, view.shape)
        self.P.dma("sp", o, view, final=True)


def colform(v):
    v = np.asarray(v, np.float32)
    return np.ascontiguousarray(v.reshape(-1, 128).T)


def rope_tables():
    t = np.arange(NLAT)
    row = (t // 64).astype(np.float32)
    col = (t % 64).astype(np.float32)
    quarter = 64
    inv = (np.float32(10000.0) ** (-np.arange(quarter, dtype=np.float32) / quarter)).astype(np.float32)
    ang = np.concatenate([row[:, None] * inv, col[:, None] * inv], -1).astype(np.float32)
    return np.ascontiguousarray(np.cos(ang).T.astype(np.float32)), np.ascontiguousarray(np.sin(ang).T.astype(np.float32))


def dist_tables():
    idx = np.arange(T)
    is_ctx = idx < LCTX
    tau_f = idx.astype(np.float64)
    tau_b = np.where(is_ctx, LCTX - 1 - idx, LCTX + NLAT - 1 - (idx - LCTX)).astype(np.float64)
    out = []
    for tau in (tau_f, tau_b):
        d = tau[None, :] - tau[:, None]
        ok = d >= 0
        ok &= ~(is_ctx[None, :] & ~is_ctx[:, None])
        out.append(np.where(ok, d, 1e9).astype(np.float32))
    return out


def na_bias_table(rpb):
    rpb = np.asarray(rpb, np.float32)
    tab = np.full((8, 16, 512, 64), -1e30, np.float32)
    rows_for_type = [0, 1, 2, 3, 10, 29, 30, 31]
    qc = np.arange(64)
    c0 = np.clip(qc - 8, 0, 48)
    for rt, r in enumerate(rows_for_type):
        r0 = min(max(r - 4, 0), 24)
        for wr in range(8):
            dr = r0 + wr - r
            kc = np.arange(64)
            dc = kc[:, None] - qc[None, :]
            ok = (kc[:, None] >= c0[None, :]) & (kc[:, None] < c0[None, :] + 16)
            dcc = np.clip(dc, -15, 15)
            vals = rpb[:, dr + 7, :][:, dcc + 15]
            blk = tab[rt, :, wr * 64:(wr + 1) * 64, :]
            blk[:, ok] = vals[:, ok]
    return tab


def prep_core_inputs(inp, core, layers, names, nb=NB):
    b0 = core * nb
    out = {}
    L = list(layers)
    for name in names:
        if name == "hT0":
            if "_h_override" in inp:
                out[name] = inp["_h_override"]
            else:
                out[name] = np.ascontiguousarray(np.concatenate(
                    [inp["ctx"][b0:b0 + nb].transpose(0, 2, 1), inp["x"][b0:b0 + nb].transpose(0, 2, 1)], axis=2))
        elif name == "cvec":
            cv = np.zeros((128, 16, 4), np.float32)
            for j in range(nb):
                cv[:, :, j] = colform(inp["c"][b0 + j])
            cv[:, :, 2] = colform(inp["c_ctx"])
            out[name] = cv
        elif name == "ada_w":
            out[name] = np.ascontiguousarray(inp["ada_w"][L])
        elif name == "ada_bT":
            out[name] = np.stack([colform(inp["ada_b"][i]) for i in L])
        elif name == "normgT":
            out[name] = np.stack([np.stack([colform(inp["norm_g"][i, w]) for w in range(2)]) for i in L])
        elif name == "final_gT":
            out[name] = colform(inp["final_g"])
        elif name == "router":
            out[name] = np.ascontiguousarray(inp["moe_router"][L])
        elif name in ("moe_w1", "moe_w3", "moe_w2"):
            out[name] = np.ascontiguousarray(inp[name][L])
        elif name == "ident":
            out[name] = np.eye(128, dtype=np.float32)
        elif name == "iota_row":
            out[name] = np.ascontiguousarray(np.broadcast_to(np.arange(288, dtype=np.float32)[None, :], (128, 288)))
        elif name == "iota_col":
            out[name] = np.ascontiguousarray(np.arange(128, dtype=np.float32)[:, None] + 128.0 * np.arange(4, dtype=np.float32)[None, :])
        elif name == "ret_decay_b":
            out[name] = np.ascontiguousarray(np.broadcast_to(inp["ret_decay"].reshape(1, 16), (128, 16)))
        elif name in ("ret_gn_wT", "ret_gn_bT", "ml_norm_wT", "hg_norm_wT"):
            out[name] = colform(inp[name[:-1]])
        elif name == "rope_cosT":
            out[name] = rope_tables()[0]
        elif name == "rope_sinT":
            out[name] = rope_tables()[1]
        elif name == "dist_f":
            out[name] = dist_tables()[0]
        elif name == "dist_b":
            out[name] = dist_tables()[1]
        elif name == "hg_tri":
            i_ = np.arange(128)
            sm, tm = i_[:, None], i_[None, :]
            out[name] = np.stack([(sm <= tm), (sm > tm), (sm >= tm), (sm < tm)]).astype(np.float32)
        elif name == "na_tab":
            out[name] = na_bias_table(inp["na_rpb"])
        elif name == "ml_wgi":
            out[name] = np.ascontiguousarray(np.concatenate([inp["ml_wgate"][0][:, :8], inp["ml_wgate"][1][:, :8]], 1))
        elif name == "ml_wgf":
            out[name] = np.ascontiguousarray(np.concatenate([inp["ml_wgate"][0][:, 8:], inp["ml_wgate"][1][:, 8:]], 1))
        elif name == "ml_bg":
            bgt = inp["ml_bgate"]
            out[name] = np.ascontiguousarray(np.stack([np.concatenate([bgt[0][:8], bgt[1][:8]]),
                                                       np.concatenate([bgt[0][8:], bgt[1][8:]])], 1))
        elif name == "ml_c":
            mc = np.zeros((16, 2), np.float32)
            mc[:8, 0] = 1.0
            mc[8:, 0] = -1.0
            mc[8:, 1] = 1.0
            out[name] = mc
        elif name == "ml_selm":
            sm = np.zeros((16, 16, 128), np.float32)
            for r_ in range(16):
                sm[r_, r_, :] = 1.0
            out[name] = sm.reshape(16, 16 * 128)
        elif name in inp:
            out[name] = np.ascontiguousarray(inp[name])
        else:
            raise KeyError(name)
    return out


def build_program(layers, nb=NB, dbg=None, stop=None):
    nc = bass.Bass("TRN2", target_bir_lowering=False)
    es = ExitStack()
    P = Prog(nc, es)
    M = Model(P, layers, dbg, nb)
    M.stop = stop
    M.emit_all()
    P.barrier()
    P.finalize(es)
    es.close()
    return nc, M


def kernel(**inputs):
    inp = {k: np.asarray(v) for k, v in inputs.items()}
    layers = [0, 1, 2, 3]
    nc, M = build_program(layers)
    names = list(M.inputs.keys())
    per_core = ("hT0", "cvec")
    shared = prep_core_inputs(inp, 0, layers, [n for n in names if n not in per_core])
    in_maps = []
    for c in range(NCORES):
        m = dict(shared)
        m.update(prep_core_inputs(inp, c, layers, list(per_core)))
        in_maps.append(m)
    res = run_bass_kernel_spmd(nc, in_maps, core_ids=list(range(NCORES)))
    out = np.empty((NCORES * NB, NLAT, D), np.float32)
    for c in range(NCORES):
        out[c * NB:(c + 1) * NB] = res.results[c]["outT"].transpose(0, 2, 1)
    return out
```
